# Optimizing a Trainium2 kernel written in Bass

```python
import jax, jax.numpy as jnp
from jax import lax
import numpy as np

D_MODEL = 1024
BATCH = 4
SEQ = 4096
DEPTH = 4

N_MIXERS = 2
SB_HEADS = 16
SB_HEAD_DIM = D_MODEL // SB_HEADS
SB_BLOCK = 128
RET_HEADS = 4
RET_QK_DIM = D_MODEL // RET_HEADS
RET_V_DIM = 2 * D_MODEL // RET_HEADS
RET_CHUNK = 128
ROPE_BASE = 10000.0
MLP_HIDDEN = 4 * D_MODEL
RMS_EPS = 1e-6
GN_EPS = 1e-6
N_SB_LAYERS = (DEPTH + N_MIXERS - 1) // N_MIXERS
N_RET_LAYERS = DEPTH // N_MIXERS

kernel_name = "hybrid_stickbreaking_retention_trunk"

F32 = jnp.float32


def rms_norm(x, g):
    xf = x.astype(F32)
    y = xf * lax.rsqrt(jnp.mean(xf * xf, axis=-1, keepdims=True) + RMS_EPS)
    return (y * g.astype(F32)).astype(x.dtype)


def stick_breaking_attention(h, w_in, w_out):
    b, s, _ = h.shape
    q, k, v = jnp.split(h @ w_in, 3, axis=-1)

    def heads(t):
        return t.reshape(b, s, SB_HEADS, SB_HEAD_DIM).transpose(0, 2, 1, 3).astype(F32)

    q, k, v = heads(q), heads(k), heads(v)
    scale = SB_HEAD_DIM ** -0.5
    outs = []
    for blk in range(s // SB_BLOCK):
        start = blk * SB_BLOCK
        end = start + SB_BLOCK
        qb = q[:, :, start:end]
        kp = k[:, :, :end]
        vp = v[:, :, :end]
        z = jnp.einsum('bhqd,bhkd->bhqk', qb, kp) * scale
        t_idx = start + jnp.arange(SB_BLOCK)[:, None]
        s_idx = jnp.arange(end)[None, :]
        mask = s_idx < t_idx
        log_keep = jnp.where(mask, jax.nn.log_sigmoid(-z), 0.0)
        later = lax.cumsum(log_keep, axis=3, reverse=True) - log_keep
        a = jnp.where(mask, jnp.exp(jax.nn.log_sigmoid(z) + later), 0.0)
        outs.append(jnp.einsum('bhqk,bhkd->bhqd', a, vp))
    o = jnp.concatenate(outs, axis=2)
    o = o.transpose(0, 2, 1, 3).reshape(b, s, D_MODEL).astype(h.dtype)
    return o @ w_out


def rotary(x, pos):
    half = x.shape[-1] // 2
    inv_freq = 1.0 / (ROPE_BASE ** jnp.linspace(0.0, 1.0, half, dtype=F32))
    ang = pos.astype(F32)[:, None] * inv_freq[None, :]
    cos, sin = jnp.cos(ang), jnp.sin(ang)
    x1, x2 = x[..., :half], x[..., half:]
    return jnp.concatenate([x1 * cos - x2 * sin, x1 * sin + x2 * cos], axis=-1)


def retention(h, w_in, w_out):
    b, s, _ = h.shape
    c = RET_CHUNK
    nc = s // c
    q, k, v, g = jnp.split(h @ w_in, [D_MODEL, 2 * D_MODEL, 4 * D_MODEL], axis=-1)
    q = q.reshape(b, s, RET_HEADS, RET_QK_DIM).transpose(0, 2, 1, 3).astype(F32)
    k = k.reshape(b, s, RET_HEADS, RET_QK_DIM).transpose(0, 2, 1, 3).astype(F32)
    v = v.reshape(b, s, RET_HEADS, RET_V_DIM).transpose(0, 2, 1, 3).astype(F32)
    pos = jnp.arange(s)
    q = rotary(q, pos)
    k = rotary(k, pos) * (RET_QK_DIM ** -0.5)

    log_gamma = jnp.log1p(-jnp.exp2(-5.0 - jnp.arange(RET_HEADS, dtype=F32)))
    idx = jnp.arange(c, dtype=F32)
    rel = idx[:, None] - idx[None, :]
    decay_in = jnp.where(rel >= 0, jnp.exp(log_gamma[:, None, None] * jnp.maximum(rel, 0.0)), 0.0)
    q_decay = jnp.exp(log_gamma[:, None] * (idx + 1.0))[None, :, :, None]
    k_decay = jnp.exp(log_gamma[:, None] * (c - 1.0 - idx))[None, :, :, None]
    chunk_decay = jnp.exp(log_gamma * c)[None, :, None, None]

    qc = q.reshape(b, RET_HEADS, nc, c, RET_QK_DIM)
    kc = k.reshape(b, RET_HEADS, nc, c, RET_QK_DIM)
    vc = v.reshape(b, RET_HEADS, nc, c, RET_V_DIM)

    scores = jnp.einsum('bhnid,bhnjd->bhnij', qc, kc) * decay_in[None, :, None]
    inner = jnp.einsum('bhnij,bhnje->bhnie', scores, vc)

    def step(state, xs):
        q_n, k_n, v_n = xs
        cross = jnp.einsum('bhid,bhde->bhie', q_n, state) * q_decay
        state = state * chunk_decay + jnp.einsum('bhjd,bhje->bhde', k_n * k_decay, v_n)
        return state, cross

    state0 = jnp.zeros((b, RET_HEADS, RET_QK_DIM, RET_V_DIM), F32)
    xs = (qc.transpose(2, 0, 1, 3, 4), kc.transpose(2, 0, 1, 3, 4), vc.transpose(2, 0, 1, 3, 4))
    _, cross = lax.scan(step, state0, xs)
    o = (inner + cross.transpose(1, 2, 0, 3, 4)).reshape(b, RET_HEADS, s, RET_V_DIM)

    mu = jnp.mean(o, axis=-1, keepdims=True)
    var = jnp.mean(jnp.square(o - mu), axis=-1, keepdims=True)
    o = (o - mu) * lax.rsqrt(var + GN_EPS)
    o = o.transpose(0, 2, 1, 3).reshape(b, s, RET_HEADS * RET_V_DIM)
    y = jax.nn.silu(g.astype(F32)) * o
    return y.astype(h.dtype) @ w_out


def squared_relu_mlp(h, w_up, w_down):
    u = jax.nn.relu(h @ w_up)
    return (u * u) @ w_down


def setup_inputs(seed: int = 0) -> dict:
    key = jax.random.key(seed)
    ks = jax.random.split(key, 11)

    def w(k, shape, fan_in):
        return jax.random.normal(k, shape, F32) * (fan_in ** -0.5)

    def gain(k, shape):
        return 1.0 + 0.05 * jax.random.normal(k, shape, F32)

    return {
        "x": jax.random.normal(ks[0], (BATCH, SEQ, D_MODEL), F32),
        "w_sb_in": w(ks[1], (N_SB_LAYERS, D_MODEL, 3 * D_MODEL), D_MODEL),
        "w_sb_out": w(ks[2], (N_SB_LAYERS, D_MODEL, D_MODEL), D_MODEL),
        "w_ret_in": w(ks[3], (N_RET_LAYERS, D_MODEL, 6 * D_MODEL), D_MODEL),
        "w_ret_out": w(ks[4], (N_RET_LAYERS, 2 * D_MODEL, D_MODEL), 2 * D_MODEL),
        "g_mix": gain(ks[5], (DEPTH, D_MODEL)),
        "g_mlp": gain(ks[6], (DEPTH, D_MODEL)),
        "w_mlp_up": w(ks[7], (DEPTH, D_MODEL, MLP_HIDDEN), D_MODEL),
        "w_mlp_down": w(ks[8], (DEPTH, MLP_HIDDEN, D_MODEL), MLP_HIDDEN),
        "g_final": gain(ks[9], (D_MODEL,)),
    }


def reference(x, w_sb_in, w_sb_out, w_ret_in, w_ret_out, g_mix, g_mlp, w_mlp_up, w_mlp_down, g_final):
    h = x
    for layer in range(DEPTH):
        hn = rms_norm(h, g_mix[layer])
        i = layer // N_MIXERS
        if layer % N_MIXERS == 0:
            h = h + stick_breaking_attention(hn, w_sb_in[i], w_sb_out[i])
        else:
            h = h + retention(hn, w_ret_in[i], w_ret_out[i])
        h = h + squared_relu_mlp(rms_norm(h, g_mlp[layer]), w_mlp_up[layer], w_mlp_down[layer])
    return rms_norm(h, g_final)
```

```python
import contextlib
import numpy as np
import concourse.bass as bass
import concourse.mybir as mybir
from concourse.bass_utils import run_bass_kernel_spmd

F32 = mybir.dt.float32
BF16 = mybir.dt.bfloat16
AF = mybir.ActivationFunctionType
ALU = mybir.AluOpType

D = 1024
S = 4096
NB = 4
DEPTH = 4
OWN = 2048
TT = 512
NOWN = OWN // TT
NALL = S // TT
HID = 4096
GROUPS = [[0, 1], [2, 3], [4, 5], [6, 7]]
RMS_EPS = 1e-6
SB_TMAX = 8
SB_ATT = True
SB_RS = True
SB_LVL = 9
GN_EPS = 1e-6

C_TRI, C_ONES, C_MLE, C_ID, C_MASK = 0, 128, 256, 384, 512
C_UTRI = 512 + 4 * 512
NCB = C_UTRI + 128
F_ID, F_G, F_SCW, F_GC, F_EPS = 0, 128, 200, 202, 204
NCF = 206


class Buf:
    __slots__ = ("name", "w", "r", "dsem", "dcnt", "key", "excl")

    def __init__(self, name, excl=False):
        self.name = name
        self.excl = excl
        self.w = None
        self.r = {}
        self.dsem = None
        self.dcnt = 0


class KB:
    def __init__(self, nc):
        self.nc = nc
        self.E = {"pe": nc.tensor, "act": nc.scalar, "dve": nc.vector, "pool": nc.gpsimd, "sp": nc.sync}
        self.sem = {e: nc.alloc_semaphore("c_" + e) for e in ("pe", "act", "dve", "pool")}
        self.cnt = {e: 0 for e in self.sem}
        self.seen = {e: {} for e in self.E}
        self.latest = {}
        self.nsem = 4

    def _deps(self, R, W, nowaw=False):
        toks = []
        for b in R:
            if b.w is not None:
                toks.append(b.w)
            if b.excl:
                toks.extend(b.r.values())
        for b in W:
            if b.w is not None and not (nowaw and b.w[3] == "dma"):
                toks.append(b.w)
            toks.extend(b.r.values())
        return toks

    def _wait(self, e, toks):
        for (key, sem, val, prod) in toks:
            if prod == "pe" and e == "pe":
                continue
            if self.seen[e].get(key, 0) < val:
                self.E[e].wait_ge(sem, val)
                self.seen[e][key] = val

    def _commit(self, tok, R, W):
        self.latest[tok[0]] = tok
        for b in R:
            b.r[tok[0]] = tok
        for b in W:
            b.w = tok
            b.r = {}

    def op(self, e, fn, R=(), W=()):
        self._wait(e, self._deps(R, W))
        inst = fn(self.E[e])
        self.cnt[e] += 1
        inst.then_inc(self.sem[e], 1)
        self._commit(("c_" + e, self.sem[e], self.cnt[e], e), R, W)

    def _dsem(self, dst):
        if dst.dsem is None:
            self.nsem += 1
            dst.key = "d%d_%s" % (self.nsem, dst.name)
            dst.dsem = self.nc.alloc_semaphore(dst.key)
        return dst.dsem

    def dma(self, q, out, in_, R=(), W=(), nowaw=False):
        dst = W[0]
        self._wait(q, self._deps(R, W, nowaw))
        sem = self._dsem(dst)
        inst = self.E[q].dma_start(out=out, in_=in_)
        dst.dcnt += 16
        inst.then_inc(sem, 16)
        self._commit((dst.key, sem, dst.dcnt, "dma"), R, W)

    def cc(self, kind, alu, in_ap, out_ap, R=(), W=()):
        dst = W[0]
        self._wait("pool", self._deps(R, W))
        sem = self._dsem(dst)
        inst = self.nc.gpsimd.collective_compute(kind, alu, replica_groups=GROUPS, ins=[in_ap], outs=[out_ap])
        dst.dcnt += 1
        inst.then_inc(sem)
        self._commit((dst.key, sem, dst.dcnt, "dma"), R, W)

    def barrier(self, engines=("pe", "act", "dve", "pool", "sp")):
        toks = list(self.latest.values())
        for e in engines:
            self._wait(e, toks)


def build_program(stop_after=None, debug=False):
    nc = bass.Bass("TRN2", target_bir_lowering=False)
    k = KB(nc)

    _uid = [0]

    def un(name):
        _uid[0] += 1
        return "%s_%d" % (name, _uid[0])

    def dram_in(name, shape, dt=F32):
        return nc.dram_tensor(name, shape, dt, kind="ExternalInput")

    x_d = dram_in("x", [OWN, D])
    wsi_d = dram_in("w_sb_in", [2, D, 1536])
    wso_d = dram_in("w_sb_out", [2, 512, D])
    wri_d = dram_in("w_ret_in", [2, D, 3072])
    wro_d = dram_in("w_ret_out", [2, 1024, D])
    wup_d = dram_in("w_up", [DEPTH, D, HID])
    wdn_d = dram_in("w_dn", [DEPTH, HID, D])
    cb_d = dram_in("cb", [128, NCB])
    cf_d = dram_in("cf", [128, NCF])
    cos_d = dram_in("cosT", [128, S])
    sin_d = dram_in("sinT", [128, S])
    y_d = nc.dram_tensor("y", [OWN, D], F32, kind="ExternalOutput")

    hres_d = nc.dram_tensor("hres", [NOWN, 128, 8, TT], F32)
    xb_d = [nc.dram_tensor(f"xb{j}", [D, TT], BF16) for j in range(NOWN)]
    yb_d = [nc.dram_tensor(f"yb{j}", [2 * D, TT], BF16) for j in range(NOWN)]
    pb_d = [nc.dram_tensor(f"pb{m}", [2 * 8 * 128, 256], F32) for m in range(8)]
    rb_d = [nc.dram_tensor(f"rb{m}", [8 * 128, 256], F32) for m in range(8)]
    B_hres = [Buf(f"hres{j}") for j in range(NOWN)]
    B_xb = [Buf(f"xb{j}") for j in range(NOWN)]
    B_yb = [Buf(f"yb{j}") for j in range(NOWN)]
    B_pb = [Buf(f"pb{m}") for m in range(8)]
    B_rb = [Buf(f"rb{m}") for m in range(8)]

    cb = nc.alloc_sbuf_tensor("cb_s", [128, NCB], BF16)
    cf = nc.alloc_sbuf_tensor("cf_s", [128, NCF], F32)
    B_cb, B_cf = Buf("cb"), Buf("cf")
    k.dma("pool", cb[:, :], cb_d[:, :], W=[B_cb])
    k.dma("sp", cf[:, :], cf_d[:, :], W=[B_cf])
    TRI = cb[:, C_TRI:C_TRI + 128]
    ONES = cb[:, C_ONES:C_ONES + 128]
    UTRI = cb[:, C_UTRI:C_UTRI + 128]
    MLE = cb[:, C_MLE:C_MLE + 128]
    IDB = cb[:, C_ID:C_ID + 128]
    IDF = cf[:, F_ID:F_ID + 128]

    def load_w(dst, src2d, nk, B_dst):
        for kc in range(nk):
            k.dma("pool", dst[:, kc, :], src2d[kc * 128:(kc + 1) * 128, :], W=[B_dst], nowaw=True)

    def maskd(d):
        return cb[:, C_MASK + d * 512:C_MASK + (d + 1) * 512]

    PSB = [Buf(f"ps{i}", excl=True) for i in range(8)]

    def rmsnorm(h, B_h, gidx, out, B_out, sq, B_sq, tmp, B_tmp, rstd, B_rstd, ps, B_ps):
        k.op("act", lambda e: e.activation(out=sq[:, :, :], in_=h[:, :, :], func=AF.Square),
             R=[B_h], W=[B_sq])
        for c in range(8):
            k.op("pe", lambda e, c=c: e.matmul(ps[:, :], lhsT=ONES, rhs=sq[:, c, :], start=(c == 0), stop=(c == 7)),
                 R=[B_sq, B_cb], W=[B_ps])
        k.op("act", lambda e: e.activation(out=tmp[:, :], in_=ps[:, :], func=AF.Ln, scale=1.0 / D, bias=RMS_EPS),
             R=[B_ps], W=[B_tmp])
        k.op("act", lambda e: e.activation(out=rstd[:, :], in_=tmp[:, :], func=AF.Exp, scale=-0.5),
             R=[B_tmp], W=[B_rstd])
        for c in range(8):
            k.op("dve", lambda e, c=c: e.scalar_tensor_tensor(
                out=out[:, c, :], in0=h[:, c, :], scalar=cf[:, F_G + gidx * 8 + c:F_G + gidx * 8 + c + 1],
                in1=rstd[:, :], op0=ALU.mult, op1=ALU.mult), R=[B_h, B_rstd, B_cf], W=[B_out])

    def emit_hn(j, hn, B_hn):
        k.dma("sp", xb_d[j].ap().rearrange("(c p) t -> p c t", p=128), hn[:, :, :], R=[B_hn], W=[B_xb[j]])
        k.cc("AllGather", ALU.bypass, xb_d[j].ap(), yb_d[j].ap(), R=[B_xb[j]], W=[B_yb[j]])

    def load_hn(T, hn, B_hn):
        j, r = T % NOWN, T // NOWN
        k.dma("sp", hn[:, :, :], yb_d[j][r * D:(r + 1) * D, :].rearrange("(c p) t -> p c t", p=128),
              R=[B_yb[j]], W=[B_hn])

    def store_partial(T, stage, B_stage):
        j, r = T % NOWN, T // NOWN
        for half in range(2):
            m = 2 * j + half
            k.dma("sp", pb_d[m][r * 1024:(r + 1) * 1024, :].rearrange("(c p) t -> p c t", p=128),
                  stage[:, :, half * 256:(half + 1) * 256], R=[B_stage], W=[B_pb[m]], nowaw=True)

    def reduce_scatter_all():
        for m in range(8):
            k.cc("ReduceScatter", ALU.add, pb_d[m].ap(), rb_d[m].ap(), R=[B_pb[m]], W=[B_rb[m]])

    def phase0():
        with contextlib.ExitStack() as es:
            X = es.enter_context(nc.sbuf_tensor("p0_X", [128, 4, D], F32))
            h = es.enter_context(nc.sbuf_tensor("p0_h", [128, 8, TT], F32))
            hn = es.enter_context(nc.sbuf_tensor("p0_hn", [128, 8, TT], BF16))
            sq = es.enter_context(nc.sbuf_tensor("p0_sq", [128, 8, TT], BF16))
            tmp = es.enter_context(nc.sbuf_tensor("p0_tmp", [128, TT], F32))
            rstd = es.enter_context(nc.sbuf_tensor("p0_rstd", [128, TT], F32))
            ps = [es.enter_context(nc.psum_tensor(f"p0_ps{i}", [128, TT], F32)) for i in range(3)]
            B_X, B_h, B_hn, B_sq, B_tmp, B_rstd = (Buf(n) for n in ("p0X", "p0h", "p0hn", "p0sq", "p0tmp", "p0rstd"))
            for j in range(NOWN):
                k.dma("sp", X[:, :, :], x_d[j * TT:(j + 1) * TT, :].rearrange("(tb p) f -> p tb f", p=128), W=[B_X])
                for c in range(8):
                    pb_ = ps[c % 2]
                    for tb in range(4):
                        k.op("pe", lambda e, c=c, tb=tb, pb_=pb_: e.transpose(
                            out=pb_[:, tb * 128:(tb + 1) * 128], in_=X[:, tb, c * 128:(c + 1) * 128], identity=IDF),
                            R=[B_X, B_cf], W=[PSB[c % 2]])
                    k.op("act", lambda e, c=c, pb_=pb_: e.activation(out=h[:, c, :], in_=pb_[:, :], func=AF.Copy),
                         R=[PSB[c % 2]], W=[B_h])
                k.dma("sp", hres_d[j], h[:, :, :], R=[B_h], W=[B_hres[j]])
                rmsnorm(h, B_h, 0, hn, B_hn, sq, B_sq, tmp, B_tmp, rstd, B_rstd, ps[2], PSB[2])
                emit_hn(j, hn, B_hn)
            k.barrier()

    def phase_sb(li):
        with contextlib.ExitStack() as es:
            def sb(name, shape, dt):
                return es.enter_context(nc.sbuf_tensor(un("sb_" + name), shape, dt))
            Win = sb("Win", [128, 8, 1536], BF16)
            Wo = sb("Wo", [128, 4, D], BF16)
            kT = sb("kT", [128, 4, S], BF16)
            vA = sb("vA", [128, 32, 512], BF16)
            hn = [sb(f"hn{i}", [128, 8, TT], BF16) for i in range(2)]
            qT = sb("qT", [128, 4, TT], BF16)
            Ee = [sb(f"E{i}", [128, 2 * TT], F32) for i in range(2)]
            sp = [sb(f"sp{i}", [128, 2 * TT], BF16) for i in range(2)]
            Gg = sb("G", [128, 2 * TT], F32)
            accf = sb("accf", [128, 2 * TT], F32)
            accb = [sb(f"accb{i}", [128, 2 * TT], BF16) for i in range(2)]
            At = [sb(f"At{i}", [128, 2 * TT], BF16) for i in range(2)]
            oT = sb("oT", [128, 4, TT], BF16)
            stage = sb("stage", [128, 8, TT], F32)
            ps = [es.enter_context(nc.psum_tensor(un(f"sb_ps{i}"), [128, TT], F32)) for i in range(2)]
            Zab = es.enter_context(nc.psum_tensor(un("sb_Z"), [128, 2 * TT], F32))
            Cab = es.enter_context(nc.psum_tensor(un("sb_C"), [128, 2 * TT], F32))
            Oab = es.enter_context(nc.psum_tensor(un("sb_O"), [128, 2 * TT], F32))
            B_Win, B_Wo = Buf("sbWin"), Buf("sbWo")
            B_kT = [Buf(f"kT{i}") for i in range(NALL)]
            B_v = [Buf(f"v{i}") for i in range(NALL)]
            B_hn = [Buf("sbhn0"), Buf("sbhn1")]
            B_qT, B_nqT, B_oT, B_stage = Buf("qT"), Buf("nqT"), Buf("oT"), Buf("sbstage")
            B_E = [Buf("E0"), Buf("E1")]
            B_sp = [Buf("sp0"), Buf("sp1")]
            B_G = Buf("G")
            B_accf = Buf("accf")
            B_accb = [Buf("accb0"), Buf("accb1")]
            B_At = [Buf("At0"), Buf("At1")]

            load_w(Win, wsi_d[li], 8, B_Win)
            load_w(Wo, wso_d[li], 4, B_Wo)
            load_hn(0, hn[0], B_hn[0])
            pp = 0
            for T in range(SB_TMAX):
                hb, B_hb = hn[T % 2], B_hn[T % 2]
                if T + 1 < NALL:
                    load_hn(T + 1, hn[(T + 1) % 2], B_hn[(T + 1) % 2])
                for pr in range(4):
                    for which in range(2):
                        bank = pp % 2
                        pp += 1
                        col0 = which * 512 + pr * 128
                        for kc in range(8):
                            k.op("pe", lambda e, kc=kc, col0=col0, bank=bank: e.matmul(
                                ps[bank][:, :], lhsT=Win[:, kc, col0:col0 + 128], rhs=hb[:, kc, :],
                                start=(kc == 0), stop=(kc == 7)), R=[B_Win, B_hb], W=[PSB[bank]])
                        if which == 0:
                            k.op("dve", lambda e, pr=pr, bank=bank: e.tensor_copy(out=qT[:, pr, :], in_=ps[bank][:, :]),
                                 R=[PSB[bank]], W=[B_qT])
                        else:
                            k.op("act", lambda e, pr=pr, bank=bank: e.activation(
                                out=kT[:, pr, T * TT:(T + 1) * TT], in_=ps[bank][:, :], func=AF.Copy),
                                R=[PSB[bank]], W=[B_kT[T]])
                for tb in range(4 if SB_LVL >= 2 else 0):
                    bank = pp % 2
                    pp += 1
                    for kc in range(8):
                        k.op("pe", lambda e, kc=kc, tb=tb, bank=bank: e.matmul(
                            ps[bank][:, :], lhsT=hb[:, kc, tb * 128:(tb + 1) * 128], rhs=Win[:, kc, 1024:1536],
                            start=(kc == 0), stop=(kc == 7)), R=[B_Win, B_hb], W=[PSB[bank]])
                    k.op("dve", lambda e, tb=tb, bank=bank: e.tensor_copy(out=vA[:, T * 4 + tb, :], in_=ps[bank][:, :]),
                         R=[PSB[bank]], W=[B_v[T]])
                nst = 4 * T + 4
                BZ = (PSB[2], PSB[3]); BC = (PSB[4], PSB[5]); BO = (PSB[6], PSB[7])
                for pr in range(4 if SB_ATT else 0):
                    offs = (0, 64)

                    def hs(s_):
                        return slice(s_ * TT, (s_ + 1) * TT)

                    def zmm(i):
                        kb = 4 * T + 3 - i
                        for s_ in range(2):
                            o_ = offs[s_]
                            k.op("pe", lambda e: e.matmul(
                                Zab[:, hs(s_)], lhsT=kT[o_:o_ + 64, pr, kb * 128:(kb + 1) * 128],
                                rhs=qT[o_:o_ + 64, pr, :], start=True, stop=True),
                                R=[B_kT[kb // 4], B_qT], W=[BZ[s_]])

                    def act_E(i):
                        par = i % 2
                        k.op("act", lambda e: e.activation(out=Ee[par][:, :], in_=Zab[:, :], func=AF.Exp, scale=0.125),
                             R=[BZ[0], BZ[1]], W=[B_E[par]])

                    def avmm(i):
                        kb = 4 * T + 3 - i
                        par = i % 2
                        for s_ in range(2):
                            k.op("pe", lambda e: e.matmul(
                                Oab[:, hs(s_)], lhsT=vA[:, kb, pr * 128:(pr + 1) * 128], rhs=At[par][:, hs(s_)],
                                start=(i == 0), stop=(i == nst - 1)), R=[B_v[kb // 4], B_At[par]], W=[BO[s_]])

                    zmm(0)
                    act_E(0)
                    for i in range(nst):
                        kb = 4 * T + 3 - i
                        dz = kb - 4 * T
                        par = i % 2
                        if i + 1 < nst:
                            zmm(i + 1)
                        k.op("act", lambda e: e.activation(out=sp[par][:, :], in_=Ee[par][:, :], func=AF.Ln, bias=1.0),
                             R=[B_E[par]], W=[B_sp[par]])
                        if dz >= 0:
                            for s_ in range(2):
                                k.op("pool", lambda e: e.tensor_tensor(
                                    out=sp[par][:, hs(s_)], in0=sp[par][:, hs(s_)], in1=maskd(dz), op=ALU.mult),
                                    R=[B_sp[par], B_cb], W=[B_sp[par]])
                        if i + 1 < nst:
                            act_E(i + 1)
                        for s_ in range(2):
                            k.op("pe", lambda e: e.matmul(Cab[:, hs(s_)], lhsT=TRI, rhs=sp[par][:, hs(s_)],
                                                          start=True, stop=(i == 0)),
                                 R=[B_sp[par], B_cb], W=[BC[s_]])
                            if i > 0:
                                k.op("pe", lambda e: e.matmul(Cab[:, hs(s_)], lhsT=ONES, rhs=accb[par][:, hs(s_)],
                                                              start=False, stop=True),
                                     R=[B_accb[par], B_cb], W=[BC[s_]])
                        k.op("act", lambda e: e.activation(out=Gg[:, :], in_=Cab[:, :], func=AF.Exp, scale=-1.0),
                             R=[BC[0], BC[1]], W=[B_G])
                        if i > 0:
                            avmm(i - 1)
                        if i + 1 < nst:
                            if i == 0:
                                k.op("pool", lambda e: e.tensor_copy(out=accf[:, :], in_=sp[par][:, :]),
                                     R=[B_sp[par]], W=[B_accf])
                            else:
                                k.op("pool", lambda e: e.tensor_tensor(out=accf[:, :], in0=accf[:, :], in1=sp[par][:, :], op=ALU.add),
                                     R=[B_sp[par], B_accf], W=[B_accf])
                            k.op("dve", lambda e: e.tensor_copy(out=accb[1 - par][:, :], in_=accf[:, :]),
                                 R=[B_accf], W=[B_accb[1 - par]])
                        k.op("dve", lambda e: e.tensor_tensor(out=At[par][:, :], in0=Ee[par][:, :], in1=Gg[:, :], op=ALU.mult),
                             R=[B_E[par], B_G], W=[B_At[par]])
                        if dz >= 0:
                            for s_ in range(2):
                                k.op("pool", lambda e: e.tensor_tensor(
                                    out=At[par][:, hs(s_)], in0=At[par][:, hs(s_)], in1=maskd(dz), op=ALU.mult),
                                    R=[B_At[par], B_cb], W=[B_At[par]])
                    avmm(nst - 1)
                    for s_ in range(2):
                        o_ = offs[s_]
                        k.op("dve", lambda e: e.tensor_copy(out=oT[o_:o_ + 64, pr, :], in_=Oab[o_:o_ + 64, hs(s_)]),
                             R=[BO[s_]], W=[B_oT])
                for oc in range(8 if SB_LVL >= 3 else 0):
                    bank = pp % 2
                    pp += 1
                    for pc in range(4):
                        k.op("pe", lambda e, oc=oc, pc=pc, bank=bank: e.matmul(
                            ps[bank][:, :], lhsT=Wo[:, pc, oc * 128:(oc + 1) * 128], rhs=oT[:, pc, :],
                            start=(pc == 0), stop=(pc == 3)), R=[B_Wo, B_oT], W=[PSB[bank]])
                    k.op("act", lambda e, oc=oc, bank=bank: e.activation(out=stage[:, oc, :], in_=ps[bank][:, :], func=AF.Copy),
                         R=[PSB[bank]], W=[B_stage])
                if SB_LVL >= 4:
                    store_partial(T, stage, B_stage)
            if SB_RS:
                reduce_scatter_all()
            k.barrier()

    def phase_ret(li):
        with contextlib.ExitStack() as es:
            def sb(name, shape, dt):
                return es.enter_context(nc.sbuf_tensor(un("rt_" + name), shape, dt))
            Win = sb("Win", [128, 8, 3072], BF16)
            Wo = sb("Wo", [128, 8, D], BF16)
            hn = [sb(f"hn{i}", [128, 8, TT], BF16) for i in range(2)]
            cs = sb("cos", [128, TT], F32)
            sn = sb("sin", [128, TT], F32)
            qT = sb("qT", [128, 4, TT], BF16)
            kTt = sb("kT", [128, 4, TT], BF16)
            ktok = sb("ktok", [128, 4, 512], BF16)
            w = sb("w", [128, 4, 2, 512], BF16)
            sg = sb("sg", [128, 4, 2, 512], BF16)
            tA = [sb(f"tA{i}", [128, TT], F32) for i in range(4)]
            sT = sb("sT", [128, 128], BF16)
            stf = sb("stf", [128, 2, 2, 512], F32)
            stb = sb("stb", [128, 2, 2, 512], BF16)
            on = sb("on", [128, 512], F32)
            ytk = sb("ytk", [128, 1024], BF16)
            yT = sb("yT", [128, 8, TT], BF16)
            stage = sb("stage", [128, 8, TT], F32)
            st6 = sb("st6", [128, 6], F32)
            mv = sb("mv", [128, 2], F32)
            rs = sb("rs", [128, 2], F32)
            ps = [es.enter_context(nc.psum_tensor(un(f"rt_ps{i}"), [128, TT], F32)) for i in range(7)]
            pst = es.enter_context(nc.psum_tensor(un("rt_pst"), [128, 1024], BF16))
            B_Win, B_Wo = Buf("rtWin"), Buf("rtWo")
            B_hn = [Buf("rthn0"), Buf("rthn1")]
            B_cs, B_sn, B_qT, B_kT, B_ktok, B_w, B_sg = (Buf(n) for n in ("cs", "sn", "rqT", "rkT", "ktok", "w", "sg"))
            B_tA = [Buf(f"tA{i}") for i in range(4)]
            B_sT, B_on, B_ytk, B_yT, B_stage, B_st6, B_mv, B_rs = (Buf(n) for n in ("sT", "on", "ytk", "yT", "rstage", "st6", "mv", "rs"))
            B_stf = [[Buf(f"stf{a}{b}") for b in range(2)] for a in range(2)]
            B_stb = [[Buf(f"stb{a}{b}") for b in range(2)] for a in range(2)]

            load_w(Win, wri_d[li], 8, B_Win)
            load_w(Wo, wro_d[li], 8, B_Wo)
            for hl in range(2):
                for hf in range(2):
                    k.op("dve", lambda e, hl=hl, hf=hf: e.memset(stf[:, hl, hf, :], 0.0), W=[B_stf[hl][hf]])
                    k.op("dve", lambda e, hl=hl, hf=hf: e.memset(stb[:, hl, hf, :], 0.0), W=[B_stb[hl][hf]])
            load_hn(0, hn[0], B_hn[0])
            pp = 0
            for T in range(NALL):
                hb, B_hb = hn[T % 2], B_hn[T % 2]
                if T + 1 < NALL:
                    load_hn(T + 1, hn[(T + 1) % 2], B_hn[(T + 1) % 2])
                k.dma("sp", cs[:, :], cos_d[:, T * TT:(T + 1) * TT], W=[B_cs])
                k.dma("sp", sn[:, :], sin_d[:, T * TT:(T + 1) * TT], W=[B_sn])
                for which in range(2):
                    dst, B_dst = (qT, B_qT) if which == 0 else (kTt, B_kT)
                    for hl in range(2):
                        for hf in range(2):
                            col0 = which * 512 + hl * 256 + hf * 128
                            for kc in range(8):
                                k.op("pe", lambda e, kc=kc, col0=col0, hf=hf: e.matmul(
                                    ps[hf][:, :], lhsT=Win[:, kc, col0:col0 + 128], rhs=hb[:, kc, :],
                                    start=(kc == 0), stop=(kc == 7)), R=[B_Win, B_hb], W=[PSB[hf]])
                        x1, x2 = ps[0], ps[1]
                        k.op("dve", lambda e: e.tensor_tensor(out=tA[0][:, :], in0=x1[:, :], in1=cs[:, :], op=ALU.mult),
                             R=[PSB[0], B_cs], W=[B_tA[0]])
                        k.op("dve", lambda e: e.tensor_tensor(out=tA[1][:, :], in0=x2[:, :], in1=sn[:, :], op=ALU.mult),
                             R=[PSB[1], B_sn], W=[B_tA[1]])
                        k.op("dve", lambda e: e.tensor_tensor(out=tA[2][:, :], in0=x1[:, :], in1=sn[:, :], op=ALU.mult),
                             R=[PSB[0], B_sn], W=[B_tA[2]])
                        k.op("dve", lambda e: e.tensor_tensor(out=tA[3][:, :], in0=x2[:, :], in1=cs[:, :], op=ALU.mult),
                             R=[PSB[1], B_cs], W=[B_tA[3]])
                        k.op("pool", lambda e, hl=hl, dst=dst: e.tensor_tensor(
                            out=dst[:, 2 * hl, :], in0=tA[0][:, :], in1=tA[1][:, :], op=ALU.subtract),
                            R=[B_tA[0], B_tA[1]], W=[B_dst])
                        k.op("pool", lambda e, hl=hl, dst=dst: e.tensor_tensor(
                            out=dst[:, 2 * hl + 1, :], in0=tA[2][:, :], in1=tA[3][:, :], op=ALU.add),
                            R=[B_tA[2], B_tA[3]], W=[B_dst])
                for tb in range(4):
                    for ch in range(4):
                        k.op("pe", lambda e, tb=tb, ch=ch: e.transpose(
                            out=pst[:, ch * 128:(ch + 1) * 128], in_=kTt[:, ch, tb * 128:(tb + 1) * 128], identity=IDB),
                            R=[B_kT, B_cb], W=[PSB[7]])
                    for hl in range(2):
                        k.op("dve", lambda e, tb=tb, hl=hl: e.tensor_scalar(
                            out=ktok[:, tb, hl * 256:(hl + 1) * 256], in0=pst[:, hl * 256:(hl + 1) * 256],
                            scalar1=cf[:, F_GC + hl:F_GC + hl + 1], scalar2=None, op0=ALU.mult),
                            R=[PSB[7], B_cf], W=[B_ktok])
                for tb in range(4):
                    for hl in range(2):
                        bank = 2 + pp % 2
                        pp += 1
                        for kc in range(8):
                            k.op("pe", lambda e, kc=kc, tb=tb, hl=hl, bank=bank: e.matmul(
                                ps[bank][:, :], lhsT=hb[:, kc, tb * 128:(tb + 1) * 128],
                                rhs=Win[:, kc, 1024 + hl * 512:1024 + (hl + 1) * 512],
                                start=(kc == 0), stop=(kc == 7)), R=[B_Win, B_hb], W=[PSB[bank]])
                        k.op("dve", lambda e, tb=tb, hl=hl, bank=bank: e.tensor_scalar(
                            out=w[:, tb, hl, :], in0=ps[bank][:, :], scalar1=cf[:, F_SCW + hl:F_SCW + hl + 1],
                            scalar2=None, op0=ALU.mult), R=[PSB[bank], B_cf], W=[B_w])
                for tb in range(4):
                    for hl in range(2):
                        bank = 2 + pp % 2
                        pp += 1
                        for kc in range(8):
                            k.op("pe", lambda e, kc=kc, tb=tb, hl=hl, bank=bank: e.matmul(
                                ps[bank][:, :], lhsT=hb[:, kc, tb * 128:(tb + 1) * 128],
                                rhs=Win[:, kc, 2048 + hl * 512:2048 + (hl + 1) * 512],
                                start=(kc == 0), stop=(kc == 7)), R=[B_Win, B_hb], W=[PSB[bank]])
                        ti = pp % 2
                        k.op("act", lambda e, bank=bank, ti=ti: e.activation(out=tA[ti][:, :], in_=ps[bank][:, :], func=AF.Exp, scale=-1.0),
                             R=[PSB[bank]], W=[B_tA[ti]])
                        k.op("pool", lambda e, ti=ti: e.tensor_scalar(
                            out=tA[ti][:, :], in0=tA[ti][:, :], scalar1=1.0, scalar2=None, op0=ALU.add),
                            R=[B_tA[ti]], W=[B_tA[ti]])
                        k.op("dve", lambda e, ti=ti: e.reciprocal(out=tA[ti][:, :], in_=tA[ti][:, :]),
                             R=[B_tA[ti]], W=[B_tA[ti]])
                        k.op("dve", lambda e, tb=tb, hl=hl, bank=bank, ti=ti: e.tensor_tensor(
                            out=sg[:, tb, hl, :], in0=ps[bank][:, :], in1=tA[ti][:, :], op=ALU.mult),
                            R=[PSB[bank], B_tA[ti]], W=[B_sg])
                for tb in range(4):
                    tsl = slice(tb * 128, (tb + 1) * 128)
                    for hl in range(2):
                        for hf in range(2):
                            k.op("pe", lambda e, hl=hl, hf=hf: e.matmul(
                                ps[4][:, 0:128], lhsT=kTt[:, 2 * hl + hf, tsl], rhs=qT[:, 2 * hl + hf, tsl],
                                start=(hf == 0), stop=(hf == 1)), R=[B_kT, B_qT], W=[PSB[4]])
                        k.op("dve", lambda e: e.tensor_tensor(out=sT[:, :], in0=ps[4][:, 0:128], in1=MLE, op=ALU.mult),
                             R=[PSB[4], B_cb], W=[B_sT])
                        for hf in range(2):
                            k.op("pe", lambda e, hl=hl, hf=hf: e.matmul(
                                ps[5][:, :], lhsT=qT[:, 2 * hl + hf, tsl], rhs=stb[:, hl, hf, :],
                                start=(hf == 0), stop=False), R=[B_qT, B_stb[hl][hf]], W=[PSB[5]])
                        k.op("pe", lambda e, hl=hl, tb=tb: e.matmul(
                            ps[5][:, :], lhsT=sT[:, :], rhs=w[:, tb, hl, :], start=False, stop=True),
                            R=[B_sT, B_w], W=[PSB[5]])
                        for hf in range(2):
                            k.op("pe", lambda e, hl=hl, hf=hf, tb=tb: e.matmul(
                                ps[6][:, :], lhsT=ktok[:, tb, hl * 256 + hf * 128:hl * 256 + (hf + 1) * 128],
                                rhs=w[:, tb, hl, :], start=True, stop=True), R=[B_ktok, B_w], W=[PSB[6]])
                            k.op("dve", lambda e, hl=hl, hf=hf: e.scalar_tensor_tensor(
                                out=stf[:, hl, hf, :], in0=stf[:, hl, hf, :], scalar=cf[:, F_GC + hl:F_GC + hl + 1],
                                in1=ps[6][:, :], op0=ALU.mult, op1=ALU.add),
                                R=[PSB[6], B_stf[hl][hf], B_cf], W=[B_stf[hl][hf]])
                            k.op("act", lambda e, hl=hl, hf=hf: e.activation(out=stb[:, hl, hf, :], in_=stf[:, hl, hf, :], func=AF.Copy),
                                 R=[B_stf[hl][hf]], W=[B_stb[hl][hf]])
                        k.op("dve", lambda e: e.bn_stats(out=st6[:, :], in_=ps[5][:, :]), R=[PSB[5]], W=[B_st6])
                        k.op("dve", lambda e: e.bn_aggr(out=mv[:, :], in_=st6[:, :]), R=[B_st6], W=[B_mv])
                        k.op("dve", lambda e, hl=hl: e.tensor_tensor(
                            out=rs[:, 0:1], in0=mv[:, 1:2], in1=cf[:, F_EPS + hl:F_EPS + hl + 1], op=ALU.add),
                            R=[B_mv, B_cf], W=[B_rs])
                        k.op("act", lambda e: e.activation(out=rs[:, 1:2], in_=rs[:, 0:1], func=AF.Ln), R=[B_rs], W=[B_rs])
                        k.op("act", lambda e: e.activation(out=rs[:, 0:1], in_=rs[:, 1:2], func=AF.Exp, scale=-0.5), R=[B_rs], W=[B_rs])
                        k.op("dve", lambda e: e.tensor_scalar(
                            out=on[:, :], in0=ps[5][:, :], scalar1=mv[:, 0:1], scalar2=rs[:, 0:1],
                            op0=ALU.subtract, op1=ALU.mult), R=[PSB[5], B_mv, B_rs], W=[B_on])
                        k.op("pool", lambda e, hl=hl, tb=tb: e.tensor_tensor(
                            out=ytk[:, hl * 512:(hl + 1) * 512], in0=on[:, :], in1=sg[:, tb, hl, :], op=ALU.mult),
                            R=[B_on, B_sg], W=[B_ytk])
                    for fc in range(8):
                        k.op("pe", lambda e, fc=fc: e.transpose(
                            out=pst[:, fc * 128:(fc + 1) * 128], in_=ytk[:, fc * 128:(fc + 1) * 128], identity=IDB),
                            R=[B_ytk, B_cb], W=[PSB[7]])
                    k.op("act", lambda e, tb=tb: e.activation(
                        out=yT[:, :, tb * 128:(tb + 1) * 128],
                        in_=pst[:, :].rearrange("p (c t) -> p c t", c=8), func=AF.Copy),
                        R=[PSB[7]], W=[B_yT])
                for oc in range(8):
                    bank = pp % 2
                    pp += 1
                    for fc in range(8):
                        k.op("pe", lambda e, oc=oc, fc=fc, bank=bank: e.matmul(
                            ps[bank][:, :], lhsT=Wo[:, fc, oc * 128:(oc + 1) * 128], rhs=yT[:, fc, :],
                            start=(fc == 0), stop=(fc == 7)), R=[B_Wo, B_yT], W=[PSB[bank]])
                    k.op("act", lambda e, oc=oc, bank=bank: e.activation(out=stage[:, oc, :], in_=ps[bank][:, :], func=AF.Copy),
                         R=[PSB[bank]], W=[B_stage])
                store_partial(T, stage, B_stage)
            reduce_scatter_all()
            k.barrier()

    def phase_mlp(layer):
        last = layer == DEPTH - 1
        with contextlib.ExitStack() as es:
            def sb(name, shape, dt):
                return es.enter_context(nc.sbuf_tensor(un("ml_" + name), shape, dt))
            h = sb("h", [128, 8, TT], F32)
            dl = sb("d", [128, 8, TT], F32)
            hn = sb("hn", [128, 8, TT], BF16)
            sq = sb("sq", [128, 8, TT], BF16)
            tmp = sb("tmp", [128, TT], F32)
            rstd = sb("rstd", [128, TT], F32)
            u = sb("u", [128, 32, TT], BF16)
            rl = [sb(f"rl{i}", [128, TT], F32) for i in range(2)]
            wup = [sb(f"wup{i}", [128, 8, 1024], BF16) for i in range(2)]
            wdn = [sb(f"wdn{i}", [128, 32, 256], BF16) for i in range(2)]
            ps = [es.enter_context(nc.psum_tensor(un(f"ml_ps{i}"), [128, TT], F32)) for i in range(7)]
            B_h, B_d, B_hn, B_sq, B_tmp, B_rstd, B_u = (Buf(n) for n in ("mh", "md", "mhn", "msq", "mtmp", "mrstd", "mu"))
            B_rl = [Buf("rl0"), Buf("rl1")]
            B_wup = [Buf("wup0"), Buf("wup1")]
            B_wdn = [Buf("wdn0"), Buf("wdn1")]
            wi = 0
            wj = 0
            pp = 0
            for j in range(NOWN):
                k.dma("sp", h[:, :, :], hres_d[j], R=[B_hres[j]], W=[B_h])
                for half in range(2):
                    m = 2 * j + half
                    k.dma("sp", dl[:, :, half * 256:(half + 1) * 256],
                          rb_d[m].ap().rearrange("(c p) t -> p c t", p=128), R=[B_rb[m]], W=[B_d], nowaw=True)
                k.op("pool", lambda e: e.tensor_tensor(out=h[:, :, :], in0=h[:, :, :], in1=dl[:, :, :], op=ALU.add),
                     R=[B_h, B_d], W=[B_h])
                rmsnorm(h, B_h, 4 + layer, hn, B_hn, sq, B_sq, tmp, B_tmp, rstd, B_rstd, ps[6], PSB[6])
                for q in range(4):
                    wb, B_wb = wup[wi % 2], B_wup[wi % 2]
                    wi += 1
                    load_w(wb, wup_d[layer][:, q * 1024:(q + 1) * 1024], 8, B_wb)
                    for hcl in range(8):
                        hc = q * 8 + hcl
                        bank = pp % 2
                        pp += 1
                        for kc in range(8):
                            k.op("pe", lambda e, kc=kc, hcl=hcl, bank=bank, wb=wb: e.matmul(
                                ps[bank][:, :], lhsT=wb[:, kc, hcl * 128:(hcl + 1) * 128], rhs=hn[:, kc, :],
                                start=(kc == 0), stop=(kc == 7)), R=[B_wb, B_hn], W=[PSB[bank]])
                        k.op("act", lambda e, bank=bank: e.activation(out=rl[bank][:, :], in_=ps[bank][:, :], func=AF.Relu),
                             R=[PSB[bank]], W=[B_rl[bank]])
                        k.op("dve", lambda e, bank=bank, hc=hc: e.tensor_tensor(
                            out=u[:, hc, :], in0=rl[bank][:, :], in1=rl[bank][:, :], op=ALU.mult),
                            R=[B_rl[bank]], W=[B_u])
                for o in range(4):
                    wb, B_wb = wdn[wj % 2], B_wdn[wj % 2]
                    wj += 1
                    load_w(wb, wdn_d[layer][:, o * 256:(o + 1) * 256], 32, B_wb)
                    for ol in range(2):
                        oc = o * 2 + ol
                        bank = 2 + pp % 2
                        pp += 1
                        for hc in range(32):
                            k.op("pe", lambda e, hc=hc, ol=ol, bank=bank, wb=wb: e.matmul(
                                ps[bank][:, :], lhsT=wb[:, hc, ol * 128:(ol + 1) * 128], rhs=u[:, hc, :],
                                start=(hc == 0), stop=(hc == 31)), R=[B_wb, B_u], W=[PSB[bank]])
                        k.op("dve", lambda e, oc=oc, bank=bank: e.tensor_tensor(
                            out=h[:, oc, :], in0=h[:, oc, :], in1=ps[bank][:, :], op=ALU.add),
                            R=[PSB[bank], B_h], W=[B_h])
                if not last:
                    k.dma("sp", hres_d[j], h[:, :, :], R=[B_h], W=[B_hres[j]])
                    rmsnorm(h, B_h, layer + 1, hn, B_hn, sq, B_sq, tmp, B_tmp, rstd, B_rstd, ps[6], PSB[6])
                    emit_hn(j, hn, B_hn)
                else:
                    rmsnorm(h, B_h, 8, dl, B_d, sq, B_sq, tmp, B_tmp, rstd, B_rstd, ps[6], PSB[6])
                    for tb in range(4):
                        for half in range(2):
                            bank = 4 + half
                            for cl in range(4):
                                c = half * 4 + cl
                                k.op("pe", lambda e, tb=tb, c=c, cl=cl, bank=bank: e.transpose(
                                    out=ps[bank][:, cl * 128:(cl + 1) * 128], in_=dl[:, c, tb * 128:(tb + 1) * 128],
                                    identity=IDF), R=[B_d, B_cf], W=[PSB[bank]])
                            k.op("act", lambda e, tb=tb, half=half, bank=bank: e.activation(
                                out=h[:, tb * 2 + half, :], in_=ps[bank][:, :], func=AF.Copy),
                                R=[PSB[bank]], W=[B_h])
                    k.dma("sp", y_d[j * TT:(j + 1) * TT, :].rearrange("(tb p) f -> p tb f", p=128),
                          h[:, :, :].rearrange("p (tb x) f -> p tb (x f)", x=2), R=[B_h], W=[B_y])
            k.barrier()

    B_y = Buf("yout")
    phases = [("p0", phase0)]
    for layer in range(DEPTH):
        if layer % 2 == 0:
            phases.append((f"mix{layer}", lambda layer=layer: phase_sb(layer // 2)))
        else:
            phases.append((f"mix{layer}", lambda layer=layer: phase_ret(layer // 2)))
        phases.append((f"mlp{layer}", lambda layer=layer: phase_mlp(layer)))
    for name, fn in phases:
        fn()
        if stop_after == name:
            break
    if debug:
        dbg_h = nc.dram_tensor("dbg_h", [NOWN, 128, 8, TT], F32, kind="ExternalOutput")
        dbg_yb = nc.dram_tensor("dbg_yb", [NOWN, 2 * D, TT], BF16, kind="ExternalOutput")
        dbg_rb = nc.dram_tensor("dbg_rb", [8, 8 * 128, 256], F32, kind="ExternalOutput")
        dbg_pb = nc.dram_tensor("dbg_pb", [8, 16 * 128, 256], F32, kind="ExternalOutput")
        B_dbg = Buf("dbg")
        k.barrier(engines=("sp",))
        for j in range(NOWN):
            k.dma("sp", dbg_h[j], hres_d[j], W=[B_dbg], nowaw=True)
            k.dma("sp", dbg_yb[j], yb_d[j].ap(), W=[B_dbg], nowaw=True)
        for m in range(8):
            k.dma("sp", dbg_rb[m], rb_d[m].ap(), W=[B_dbg], nowaw=True)
            k.dma("sp", dbg_pb[m], pb_d[m].ap(), W=[B_dbg], nowaw=True)
    k.barrier(engines=("sp",))
    return nc


def _consts(p):
    j = np.arange(128)
    cbm = np.zeros((128, NCB), np.float32)
    cbm[:, C_TRI:C_TRI + 128] = (j[:, None] >= j[None, :])
    cbm[:, C_ONES:C_ONES + 128] = 1.0
    cbm[:, C_MLE:C_MLE + 128] = (j[:, None] <= j[None, :])
    cbm[:, C_ID:C_ID + 128] = np.eye(128)
    t = np.arange(512)
    for d in range(4):
        cbm[:, C_MASK + d * 512:C_MASK + (d + 1) * 512] = (j[:, None] + 128 * d < t[None, :])
    cbm[:, C_UTRI:C_UTRI + 128] = (j[:, None] < j[None, :])
    return cbm


def _cf(p, g_mix, g_mlp, g_final):
    cfm = np.zeros((128, NCF), np.float32)
    cfm[:, F_ID:F_ID + 128] = np.eye(128)
    gains = np.concatenate([g_mix, g_mlp, g_final[None, :]], axis=0)
    cfm[:, F_G:F_G + 72] = gains.reshape(9, 8, 128).transpose(2, 0, 1).reshape(128, 72)
    j = np.arange(128, dtype=np.float64)
    for hl in range(2):
        hd = 2 * p + hl
        lg = np.log1p(-np.exp2(-5.0 - hd))
        cfm[:, F_SCW + hl] = np.exp(lg * (-1.0 - j)) / 16.0
        cfm[:, F_GC + hl] = np.exp(lg * 128.0)
        cfm[:, F_EPS + hl] = GN_EPS * np.exp(lg * (-2.0 * (j + 1.0)))
    return cfm


def _rope_tables():
    half = 128
    inv_freq = (1.0 / (10000.0 ** np.linspace(0.0, 1.0, half, dtype=np.float32))).astype(np.float32)
    pos = np.arange(S, dtype=np.float32)
    ang = (inv_freq[:, None] * pos[None, :]).astype(np.float32).astype(np.float64)
    return np.cos(ang).astype(np.float32), np.sin(ang).astype(np.float32)


_NC_CACHE = {}


def make_in_maps(x, w_sb_in, w_sb_out, w_ret_in, w_ret_out, g_mix, g_mlp, w_mlp_up, w_mlp_down, g_final):
    f = lambda a: np.ascontiguousarray(np.asarray(a, dtype=np.float32))
    x, w_sb_in, w_sb_out, w_ret_in, w_ret_out = f(x), f(w_sb_in), f(w_sb_out), f(w_ret_in), f(w_ret_out)
    g_mix, g_mlp, w_mlp_up, w_mlp_down, g_final = f(g_mix), f(g_mlp), f(w_mlp_up), f(w_mlp_down), f(g_final)
    cosT, sinT = _rope_tables()
    in_maps = []
    for c in range(8):
        b, p = c // 2, c % 2
        sl = slice(512 * p, 512 * p + 512)
        wsi = np.concatenate([w_sb_in[:, :, 0:1024][:, :, sl], w_sb_in[:, :, 1024:2048][:, :, sl],
                              w_sb_in[:, :, 2048:3072][:, :, sl]], axis=2)
        wso = w_sb_out[:, sl, :]
        sl2 = slice(1024 * p, 1024 * p + 1024)
        wri = np.concatenate([w_ret_in[:, :, 0:1024][:, :, sl], w_ret_in[:, :, 1024:2048][:, :, sl],
                              w_ret_in[:, :, 2048:4096][:, :, sl2], w_ret_in[:, :, 4096:6144][:, :, sl2]], axis=2)
        wro = w_ret_out[:, sl2, :]
        in_maps.append({
            "x": f(x[b, p * OWN:(p + 1) * OWN, :]),
            "w_sb_in": f(wsi), "w_sb_out": f(wso), "w_ret_in": f(wri), "w_ret_out": f(wro),
            "w_up": w_mlp_up, "w_dn": w_mlp_down,
            "cb": _consts(p), "cf": _cf(p, g_mix, g_mlp, g_final),
            "cosT": cosT, "sinT": sinT,
        })
    return in_maps


def kernel(x, w_sb_in, w_sb_out, w_ret_in, w_ret_out, g_mix, g_mlp, w_mlp_up, w_mlp_down, g_final):
    if "nc" not in _NC_CACHE:
        _NC_CACHE["nc"] = build_program()
    nc = _NC_CACHE["nc"]
    in_maps = make_in_maps(x, w_sb_in, w_sb_out, w_ret_in, w_ret_out, g_mix, g_mlp, w_mlp_up, w_mlp_down, g_final)
    res = run_bass_kernel_spmd(nc, in_maps, core_ids=list(range(8)))
    out = np.empty((NB, S, D), np.float32)
    for c in range(8):
        b, p = c // 2, c % 2
        out[b, p * OWN:(p + 1) * OWN, :] = np.asarray(res.results[c]["y"], dtype=np.float32)
    return out
```

```python
import contextlib
import numpy as np
import concourse.bass as bass
import concourse.mybir as mybir
from concourse.bass_utils import run_bass_kernel_spmd

F32 = mybir.dt.float32
BF16 = mybir.dt.bfloat16
AF = mybir.ActivationFunctionType
ALU = mybir.AluOpType

D = 1024
S = 4096
NB = 4
DEPTH = 4
OWN = 2048
TT = 512
NOWN = OWN // TT
NALL = S // TT
HID = 4096
GROUPS = [[0, 1], [2, 3], [4, 5], [6, 7]]
RMS_EPS = 1e-6
SB_TMAX = 8
SB_ATT = True
SB_RS = True
SB_LVL = 9
GN_EPS = 1e-6

C_TRI, C_ONES, C_MLE, C_ID, C_MASK = 0, 128, 256, 384, 512
C_UTRI = 512 + 4 * 512
NCB = C_UTRI + 128
F_ID, F_G, F_SCW, F_GC, F_EPS = 0, 128, 200, 202, 204
NCF = 206


class Buf:
    __slots__ = ("name", "w", "r", "dsem", "dcnt", "key", "excl")

    def __init__(self, name, excl=False):
        self.name = name
        self.excl = excl
        self.w = None
        self.r = {}
        self.dsem = None
        self.dcnt = 0


class KB:
    def __init__(self, nc):
        self.nc = nc
        self.E = {"pe": nc.tensor, "act": nc.scalar, "dve": nc.vector, "pool": nc.gpsimd, "sp": nc.sync}
        self.sem = {e: nc.alloc_semaphore("c_" + e) for e in ("pe", "act", "dve", "pool")}
        self.cnt = {e: 0 for e in self.sem}
        self.seen = {e: {} for e in self.E}
        self.latest = {}
        self.nsem = 4

    def _deps(self, R, W, nowaw=False):
        toks = []
        for b in R:
            if b.w is not None:
                toks.append(b.w)
            if b.excl:
                toks.extend(b.r.values())
        for b in W:
            if b.w is not None and not (nowaw and b.w[3] == "dma"):
                toks.append(b.w)
            toks.extend(b.r.values())
        return toks

    def _wait(self, e, toks):
        for (key, sem, val, prod) in toks:
            if prod == "pe" and e == "pe":
                continue
            if self.seen[e].get(key, 0) < val:
                self.E[e].wait_ge(sem, val)
                self.seen[e][key] = val

    def _commit(self, tok, R, W):
        self.latest[tok[0]] = tok
        for b in R:
            b.r[tok[0]] = tok
        for b in W:
            b.w = tok
            b.r = {}

    def op(self, e, fn, R=(), W=()):
        self._wait(e, self._deps(R, W))
        inst = fn(self.E[e])
        self.cnt[e] += 1
        inst.then_inc(self.sem[e], 1)
        self._commit(("c_" + e, self.sem[e], self.cnt[e], e), R, W)

    def _dsem(self, dst):
        if dst.dsem is None:
            self.nsem += 1
            dst.key = "d%d_%s" % (self.nsem, dst.name)
            dst.dsem = self.nc.alloc_semaphore(dst.key)
        return dst.dsem

    def dma(self, q, out, in_, R=(), W=(), nowaw=False):
        dst = W[0]
        self._wait(q, self._deps(R, W, nowaw))
        sem = self._dsem(dst)
        inst = self.E[q].dma_start(out=out, in_=in_)
        dst.dcnt += 16
        inst.then_inc(sem, 16)
        self._commit((dst.key, sem, dst.dcnt, "dma"), R, W)

    def cc(self, kind, alu, in_ap, out_ap, R=(), W=()):
        dst = W[0]
        self._wait("pool", self._deps(R, W))
        sem = self._dsem(dst)
        inst = self.nc.gpsimd.collective_compute(kind, alu, replica_groups=GROUPS, ins=[in_ap], outs=[out_ap])
        dst.dcnt += 1
        inst.then_inc(sem)
        self._commit((dst.key, sem, dst.dcnt, "dma"), R, W)

    def barrier(self, engines=("pe", "act", "dve", "pool", "sp")):
        toks = list(self.latest.values())
        for e in engines:
            self._wait(e, toks)


def build_program(stop_after=None, debug=False):
    nc = bass.Bass("TRN2", target_bir_lowering=False)
    k = KB(nc)

    _uid = [0]

    def un(name):
        _uid[0] += 1
        return "%s_%d" % (name, _uid[0])

    def dram_in(name, shape, dt=F32):
        return nc.dram_tensor(name, shape, dt, kind="ExternalInput")

    x_d = dram_in("x", [OWN, D])
    wsi_d = dram_in("w_sb_in", [2, D, 1536])
    wso_d = dram_in("w_sb_out", [2, 512, D])
    wri_d = dram_in("w_ret_in", [2, D, 3072])
    wro_d = dram_in("w_ret_out", [2, 1024, D])
    wup_d = dram_in("w_up", [DEPTH, D, HID])
    wdn_d = dram_in("w_dn", [DEPTH, HID, D])
    cb_d = dram_in("cb", [128, NCB])
    cf_d = dram_in("cf", [128, NCF])
    cos_d = dram_in("cosT", [128, S])
    sin_d = dram_in("sinT", [128, S])
    y_d = nc.dram_tensor("y", [OWN, D], F32, kind="ExternalOutput")

    hres_d = nc.dram_tensor("hres", [NOWN, 128, 8, TT], F32)
    xb_d = [nc.dram_tensor(f"xb{j}", [D, TT], BF16) for j in range(NOWN)]
    yb_d = [nc.dram_tensor(f"yb{j}", [2 * D, TT], BF16) for j in range(NOWN)]
    pb_d = [nc.dram_tensor(f"pb{m}", [2 * 8 * 128, 256], F32) for m in range(8)]
    rb_d = [nc.dram_tensor(f"rb{m}", [8 * 128, 256], F32) for m in range(8)]
    B_hres = [Buf(f"hres{j}") for j in range(NOWN)]
    B_xb = [Buf(f"xb{j}") for j in range(NOWN)]
    B_yb = [Buf(f"yb{j}") for j in range(NOWN)]
    B_pb = [Buf(f"pb{m}") for m in range(8)]
    B_rb = [Buf(f"rb{m}") for m in range(8)]

    cb = nc.alloc_sbuf_tensor("cb_s", [128, NCB], BF16)
    cf = nc.alloc_sbuf_tensor("cf_s", [128, NCF], F32)
    B_cb, B_cf = Buf("cb"), Buf("cf")
    k.dma("pool", cb[:, :], cb_d[:, :], W=[B_cb])
    k.dma("sp", cf[:, :], cf_d[:, :], W=[B_cf])
    TRI = cb[:, C_TRI:C_TRI + 128]
    ONES = cb[:, C_ONES:C_ONES + 128]
    UTRI = cb[:, C_UTRI:C_UTRI + 128]
    MLE = cb[:, C_MLE:C_MLE + 128]
    IDB = cb[:, C_ID:C_ID + 128]
    IDF = cf[:, F_ID:F_ID + 128]

    def load_w(dst, src2d, nk, B_dst):
        for kc in range(nk):
            k.dma("pool", dst[:, kc, :], src2d[kc * 128:(kc + 1) * 128, :], W=[B_dst], nowaw=True)

    def maskd(d):
        return cb[:, C_MASK + d * 512:C_MASK + (d + 1) * 512]

    PSB = [Buf(f"ps{i}", excl=True) for i in range(8)]

    def rmsnorm(h, B_h, gidx, out, B_out, sq, B_sq, tmp, B_tmp, rstd, B_rstd, ps, B_ps):
        k.op("act", lambda e: e.activation(out=sq[:, :, :], in_=h[:, :, :], func=AF.Square),
             R=[B_h], W=[B_sq])
        for c in range(8):
            k.op("pe", lambda e, c=c: e.matmul(ps[:, :], lhsT=ONES, rhs=sq[:, c, :], start=(c == 0), stop=(c == 7)),
                 R=[B_sq, B_cb], W=[B_ps])
        k.op("act", lambda e: e.activation(out=tmp[:, :], in_=ps[:, :], func=AF.Ln, scale=1.0 / D, bias=RMS_EPS),
             R=[B_ps], W=[B_tmp])
        k.op("act", lambda e: e.activation(out=rstd[:, :], in_=tmp[:, :], func=AF.Exp, scale=-0.5),
             R=[B_tmp], W=[B_rstd])
        for c in range(8):
            k.op("dve", lambda e, c=c: e.scalar_tensor_tensor(
                out=out[:, c, :], in0=h[:, c, :], scalar=cf[:, F_G + gidx * 8 + c:F_G + gidx * 8 + c + 1],
                in1=rstd[:, :], op0=ALU.mult, op1=ALU.mult), R=[B_h, B_rstd, B_cf], W=[B_out])

    def emit_hn(j, hn, B_hn):
        k.dma("sp", xb_d[j].ap().rearrange("(c p) t -> p c t", p=128), hn[:, :, :], R=[B_hn], W=[B_xb[j]])
        k.cc("AllGather", ALU.bypass, xb_d[j].ap(), yb_d[j].ap(), R=[B_xb[j]], W=[B_yb[j]])

    def load_hn(T, hn, B_hn):
        j, r = T % NOWN, T // NOWN
        k.dma("sp", hn[:, :, :], yb_d[j][r * D:(r + 1) * D, :].rearrange("(c p) t -> p c t", p=128),
              R=[B_yb[j]], W=[B_hn])

    def store_partial(T, stage, B_stage):
        j, r = T % NOWN, T // NOWN
        for half in range(2):
            m = 2 * j + half
            k.dma("sp", pb_d[m][r * 1024:(r + 1) * 1024, :].rearrange("(c p) t -> p c t", p=128),
                  stage[:, :, half * 256:(half + 1) * 256], R=[B_stage], W=[B_pb[m]], nowaw=True)

    def reduce_scatter_all():
        for m in range(8):
            k.cc("ReduceScatter", ALU.add, pb_d[m].ap(), rb_d[m].ap(), R=[B_pb[m]], W=[B_rb[m]])

    def phase0():
        with contextlib.ExitStack() as es:
            X = es.enter_context(nc.sbuf_tensor("p0_X", [128, 4, D], F32))
            h = es.enter_context(nc.sbuf_tensor("p0_h", [128, 8, TT], F32))
            hn = es.enter_context(nc.sbuf_tensor("p0_hn", [128, 8, TT], BF16))
            sq = es.enter_context(nc.sbuf_tensor("p0_sq", [128, 8, TT], BF16))
            tmp = es.enter_context(nc.sbuf_tensor("p0_tmp", [128, TT], F32))
            rstd = es.enter_context(nc.sbuf_tensor("p0_rstd", [128, TT], F32))
            ps = [es.enter_context(nc.psum_tensor(f"p0_ps{i}", [128, TT], F32)) for i in range(3)]
            B_X, B_h, B_hn, B_sq, B_tmp, B_rstd = (Buf(n) for n in ("p0X", "p0h", "p0hn", "p0sq", "p0tmp", "p0rstd"))
            for j in range(NOWN):
                k.dma("sp", X[:, :, :], x_d[j * TT:(j + 1) * TT, :].rearrange("(tb p) f -> p tb f", p=128), W=[B_X])
                for c in range(8):
                    pb_ = ps[c % 2]
                    for tb in range(4):
                        k.op("pe", lambda e, c=c, tb=tb, pb_=pb_: e.transpose(
                            out=pb_[:, tb * 128:(tb + 1) * 128], in_=X[:, tb, c * 128:(c + 1) * 128], identity=IDF),
                            R=[B_X, B_cf], W=[PSB[c % 2]])
                    k.op("act", lambda e, c=c, pb_=pb_: e.activation(out=h[:, c, :], in_=pb_[:, :], func=AF.Copy),
                         R=[PSB[c % 2]], W=[B_h])
                k.dma("sp", hres_d[j], h[:, :, :], R=[B_h], W=[B_hres[j]])
                rmsnorm(h, B_h, 0, hn, B_hn, sq, B_sq, tmp, B_tmp, rstd, B_rstd, ps[2], PSB[2])
                emit_hn(j, hn, B_hn)
            k.barrier()

    def phase_sb(li):
        with contextlib.ExitStack() as es:
            def sb(name, shape, dt):
                return es.enter_context(nc.sbuf_tensor(un("sb_" + name), shape, dt))
            Win = sb("Win", [128, 8, 1536], BF16)
            Wo = sb("Wo", [128, 4, D], BF16)
            kT = sb("kT", [128, 4, S], BF16)
            vA = sb("vA", [128, 32, 512], BF16)
            hn = [sb(f"hn{i}", [128, 8, TT], BF16) for i in range(2)]
            qT = sb("qT", [128, 4, TT], BF16)
            Ee = [sb(f"E{i}", [128, 2 * TT], F32) for i in range(3)]
            sp = [sb(f"sp{i}", [128, 2 * TT], BF16) for i in range(2)]
            Gg = sb("G", [128, 2 * TT], F32)
            accf = sb("accf", [128, 2 * TT], F32)
            accb = [sb(f"accb{i}", [128, 2 * TT], BF16) for i in range(2)]
            At = [sb(f"At{i}", [128, 2 * TT], BF16) for i in range(2)]
            oT = sb("oT", [128, 4, TT], BF16)
            stage = sb("stage", [128, 8, TT], F32)
            ps = [es.enter_context(nc.psum_tensor(un(f"sb_ps{i}"), [128, TT], F32)) for i in range(2)]
            Zab = es.enter_context(nc.psum_tensor(un("sb_Z"), [128, 2 * TT], F32))
            Cab = es.enter_context(nc.psum_tensor(un("sb_C"), [128, 2 * TT], F32))
            Oab = es.enter_context(nc.psum_tensor(un("sb_O"), [128, 2 * TT], F32))
            B_Win, B_Wo = Buf("sbWin"), Buf("sbWo")
            B_kT = [Buf(f"kT{i}") for i in range(NALL)]
            B_v = [Buf(f"v{i}") for i in range(NALL)]
            B_hn = [Buf("sbhn0"), Buf("sbhn1")]
            B_qT, B_nqT, B_oT, B_stage = Buf("qT"), Buf("nqT"), Buf("oT"), Buf("sbstage")
            B_E = [Buf("E0"), Buf("E1"), Buf("E2")]
            B_sp = [Buf("sp0"), Buf("sp1")]
            B_G = Buf("G")
            B_accf = Buf("accf")
            B_accb = [Buf("accb0"), Buf("accb1")]
            B_At = [Buf("At0"), Buf("At1")]

            load_w(Win, wsi_d[li], 8, B_Win)
            load_w(Wo, wso_d[li], 4, B_Wo)
            load_hn(0, hn[0], B_hn[0])
            pp = 0
            for T in range(SB_TMAX):
                hb, B_hb = hn[T % 2], B_hn[T % 2]
                if T + 1 < NALL:
                    load_hn(T + 1, hn[(T + 1) % 2], B_hn[(T + 1) % 2])
                for pr in range(4):
                    for which in range(2):
                        bank = pp % 2
                        pp += 1
                        col0 = which * 512 + pr * 128
                        for kc in range(8):
                            k.op("pe", lambda e, kc=kc, col0=col0, bank=bank: e.matmul(
                                ps[bank][:, :], lhsT=Win[:, kc, col0:col0 + 128], rhs=hb[:, kc, :],
                                start=(kc == 0), stop=(kc == 7)), R=[B_Win, B_hb], W=[PSB[bank]])
                        if which == 0:
                            k.op("dve", lambda e, pr=pr, bank=bank: e.tensor_copy(out=qT[:, pr, :], in_=ps[bank][:, :]),
                                 R=[PSB[bank]], W=[B_qT])
                        else:
                            k.op("act", lambda e, pr=pr, bank=bank: e.activation(
                                out=kT[:, pr, T * TT:(T + 1) * TT], in_=ps[bank][:, :], func=AF.Copy),
                                R=[PSB[bank]], W=[B_kT[T]])
                for tb in range(4 if SB_LVL >= 2 else 0):
                    bank = pp % 2
                    pp += 1
                    for kc in range(8):
                        k.op("pe", lambda e, kc=kc, tb=tb, bank=bank: e.matmul(
                            ps[bank][:, :], lhsT=hb[:, kc, tb * 128:(tb + 1) * 128], rhs=Win[:, kc, 1024:1536],
                            start=(kc == 0), stop=(kc == 7)), R=[B_Win, B_hb], W=[PSB[bank]])
                    k.op("dve", lambda e, tb=tb, bank=bank: e.tensor_copy(out=vA[:, T * 4 + tb, :], in_=ps[bank][:, :]),
                         R=[PSB[bank]], W=[B_v[T]])
                nst = 4 * T + 4
                BZ = (PSB[2], PSB[3]); BC = (PSB[4], PSB[5]); BO = (PSB[6], PSB[7])
                for pr in range(4 if SB_ATT else 0):
                    offs = (0, 64)

                    def hs(s_):
                        return slice(s_ * TT, (s_ + 1) * TT)

                    def zmm(i):
                        kb = 4 * T + 3 - i
                        for s_ in range(2):
                            o_ = offs[s_]
                            k.op("pe", lambda e: e.matmul(
                                Zab[:, hs(s_)], lhsT=kT[o_:o_ + 64, pr, kb * 128:(kb + 1) * 128],
                                rhs=qT[o_:o_ + 64, pr, :], start=True, stop=True),
                                R=[B_kT[kb // 4], B_qT], W=[BZ[s_]])

                    def act_E(i):
                        k.op("act", lambda e: e.activation(out=Ee[i % 3][:, :], in_=Zab[:, :], func=AF.Exp, scale=0.125),
                             R=[BZ[0], BZ[1]], W=[B_E[i % 3]])

                    def avmm(i):
                        kb = 4 * T + 3 - i
                        par = i % 2
                        for s_ in range(2):
                            k.op("pe", lambda e: e.matmul(
                                Oab[:, hs(s_)], lhsT=vA[:, kb, pr * 128:(pr + 1) * 128], rhs=At[par][:, hs(s_)],
                                start=(i == 0), stop=(i == nst - 1)), R=[B_v[kb // 4], B_At[par]], W=[BO[s_]])

                    zmm(0)
                    act_E(0)
                    for i in range(nst):
                        kb = 4 * T + 3 - i
                        dz = kb - 4 * T
                        par = i % 2
                        if i + 1 < nst:
                            zmm(i + 1)
                        k.op("act", lambda e: e.activation(out=sp[par][:, :], in_=Ee[i % 3][:, :], func=AF.Ln, bias=1.0),
                             R=[B_E[i % 3]], W=[B_sp[par]])
                        if dz >= 0:
                            for s_ in range(2):
                                k.op("pool", lambda e: e.tensor_tensor(
                                    out=sp[par][:, hs(s_)], in0=sp[par][:, hs(s_)], in1=maskd(dz), op=ALU.mult),
                                    R=[B_sp[par], B_cb], W=[B_sp[par]])
                        if i + 1 < nst:
                            act_E(i + 1)
                        for s_ in range(2):
                            k.op("pe", lambda e: e.matmul(Cab[:, hs(s_)], lhsT=TRI, rhs=sp[par][:, hs(s_)],
                                                          start=True, stop=(i == 0)),
                                 R=[B_sp[par], B_cb], W=[BC[s_]])
                            if i > 0:
                                k.op("pe", lambda e: e.matmul(Cab[:, hs(s_)], lhsT=ONES, rhs=accb[par][:, hs(s_)],
                                                              start=False, stop=True),
                                     R=[B_accb[par], B_cb], W=[BC[s_]])
                        k.op("act", lambda e: e.activation(out=Gg[:, :], in_=Cab[:, :], func=AF.Exp, scale=-1.0),
                             R=[BC[0], BC[1]], W=[B_G])
                        if i > 0:
                            avmm(i - 1)
                        if i + 1 < nst:
                            if i == 0:
                                k.op("dve", lambda e: e.tensor_copy(out=accf[:, :], in_=sp[par][:, :]),
                                     R=[B_sp[par]], W=[B_accf])
                            else:
                                k.op("dve", lambda e: e.tensor_tensor(out=accf[:, :], in0=accf[:, :], in1=sp[par][:, :], op=ALU.add),
                                     R=[B_sp[par], B_accf], W=[B_accf])
                            k.op("dve", lambda e: e.tensor_copy(out=accb[1 - par][:, :], in_=accf[:, :]),
                                 R=[B_accf], W=[B_accb[1 - par]])
                        k.op("dve", lambda e: e.tensor_tensor(out=At[par][:, :], in0=Ee[i % 3][:, :], in1=Gg[:, :], op=ALU.mult),
                             R=[B_E[i % 3], B_G], W=[B_At[par]])
                        if dz >= 0:
                            for s_ in range(2):
                                k.op("pool", lambda e: e.tensor_tensor(
                                    out=At[par][:, hs(s_)], in0=At[par][:, hs(s_)], in1=maskd(dz), op=ALU.mult),
                                    R=[B_At[par], B_cb], W=[B_At[par]])
                    avmm(nst - 1)
                    for s_ in range(2):
                        o_ = offs[s_]
                        k.op("dve", lambda e: e.tensor_copy(out=oT[o_:o_ + 64, pr, :], in_=Oab[o_:o_ + 64, hs(s_)]),
                             R=[BO[s_]], W=[B_oT])
                for oc in range(8 if SB_LVL >= 3 else 0):
                    bank = pp % 2
                    pp += 1
                    for pc in range(4):
                        k.op("pe", lambda e, oc=oc, pc=pc, bank=bank: e.matmul(
                            ps[bank][:, :], lhsT=Wo[:, pc, oc * 128:(oc + 1) * 128], rhs=oT[:, pc, :],
                            start=(pc == 0), stop=(pc == 3)), R=[B_Wo, B_oT], W=[PSB[bank]])
                    k.op("act", lambda e, oc=oc, bank=bank: e.activation(out=stage[:, oc, :], in_=ps[bank][:, :], func=AF.Copy),
                         R=[PSB[bank]], W=[B_stage])
                if SB_LVL >= 4:
                    store_partial(T, stage, B_stage)
            if SB_RS:
                reduce_scatter_all()
            k.barrier()

    def phase_ret(li):
        with contextlib.ExitStack() as es:
            def sb(name, shape, dt):
                return es.enter_context(nc.sbuf_tensor(un("rt_" + name), shape, dt))
            Win = sb("Win", [128, 8, 3072], BF16)
            Wo = sb("Wo", [128, 8, D], BF16)
            hn = [sb(f"hn{i}", [128, 8, TT], BF16) for i in range(2)]
            csb = [sb(f"cos{i}", [128, TT], F32) for i in range(2)]
            snb = [sb(f"sin{i}", [128, TT], F32) for i in range(2)]
            qT = sb("qT", [128, 4, TT], BF16)
            kTt = sb("kT", [128, 4, TT], BF16)
            ktok = sb("ktok", [128, 4, 512], BF16)
            w = sb("w", [128, 4, 2, 512], BF16)
            sg = sb("sg", [128, 4, 2, 512], BF16)
            tA = [sb(f"tA{i}", [128, TT], F32) for i in range(4)]
            sT = [sb(f"sT{i}", [128, 128], BF16) for i in range(2)]
            stf = sb("stf", [128, 2, 2, 512], F32)
            stb = sb("stb", [128, 2, 2, 512], BF16)
            on = [sb(f"on{i}", [128, 512], F32) for i in range(2)]
            ytk = sb("ytk", [128, 1024], BF16)
            yT = sb("yT", [128, 8, TT], BF16)
            stage = sb("stage", [128, 8, TT], F32)
            st6 = [sb(f"st6{i}", [128, 6], F32) for i in range(2)]
            mv = [sb(f"mv{i}", [128, 2], F32) for i in range(2)]
            rs = [sb(f"rs{i}", [128, 2], F32) for i in range(2)]
            ps = [es.enter_context(nc.psum_tensor(un(f"rt_ps{i}"), [128, TT], F32)) for i in range(7)]
            pst = es.enter_context(nc.psum_tensor(un("rt_pst"), [128, 1024], BF16))
            B_Win, B_Wo = Buf("rtWin"), Buf("rtWo")
            B_hn = [Buf("rthn0"), Buf("rthn1")]
            B_qT, B_kT, B_ktok, B_w, B_sg = (Buf(n) for n in ("rqT", "rkT", "ktok", "w", "sg"))
            B_csb = [Buf("cs0"), Buf("cs1")]
            B_snb = [Buf("sn0"), Buf("sn1")]
            B_tA = [Buf(f"tA{i}") for i in range(4)]
            B_ytk, B_yT, B_stage = (Buf(n) for n in ("ytk", "yT", "rstage"))
            B_sT, B_on, B_st6, B_mv, B_rs = ([Buf(n + "0"), Buf(n + "1")] for n in ("sT", "on", "st6", "mv", "rs"))
            B_stf = [[Buf(f"stf{a}{b}") for b in range(2)] for a in range(2)]
            B_stb = [[Buf(f"stb{a}{b}") for b in range(2)] for a in range(2)]

            load_w(Win, wri_d[li], 8, B_Win)
            load_w(Wo, wro_d[li], 8, B_Wo)
            for hl in range(2):
                for hf in range(2):
                    k.op("dve", lambda e, hl=hl, hf=hf: e.memset(stf[:, hl, hf, :], 0.0), W=[B_stf[hl][hf]])
                    k.op("dve", lambda e, hl=hl, hf=hf: e.memset(stb[:, hl, hf, :], 0.0), W=[B_stb[hl][hf]])
            load_hn(0, hn[0], B_hn[0])
            pp = 0
            for T in range(NALL):
                hb, B_hb = hn[T % 2], B_hn[T % 2]
                if T + 1 < NALL:
                    load_hn(T + 1, hn[(T + 1) % 2], B_hn[(T + 1) % 2])
                if T == 0:
                    k.dma("sp", csb[0][:, :], cos_d[:, 0:TT], W=[B_csb[0]])
                    k.dma("sp", snb[0][:, :], sin_d[:, 0:TT], W=[B_snb[0]])
                if T + 1 < NALL:
                    k.dma("sp", csb[(T + 1) % 2][:, :], cos_d[:, (T + 1) * TT:(T + 2) * TT], W=[B_csb[(T + 1) % 2]])
                    k.dma("sp", snb[(T + 1) % 2][:, :], sin_d[:, (T + 1) * TT:(T + 2) * TT], W=[B_snb[(T + 1) % 2]])
                cs, sn, B_cs, B_sn = csb[T % 2], snb[T % 2], B_csb[T % 2], B_snb[T % 2]
                for which in range(2):
                    dst, B_dst = (qT, B_qT) if which == 0 else (kTt, B_kT)
                    for hl in range(2):
                        for hf in range(2):
                            col0 = which * 512 + hl * 256 + hf * 128
                            for kc in range(8):
                                k.op("pe", lambda e, kc=kc, col0=col0, hf=hf: e.matmul(
                                    ps[hf][:, :], lhsT=Win[:, kc, col0:col0 + 128], rhs=hb[:, kc, :],
                                    start=(kc == 0), stop=(kc == 7)), R=[B_Win, B_hb], W=[PSB[hf]])
                        x1, x2 = ps[0], ps[1]
                        k.op("dve", lambda e: e.tensor_tensor(out=tA[0][:, :], in0=x1[:, :], in1=cs[:, :], op=ALU.mult),
                             R=[PSB[0], B_cs], W=[B_tA[0]])
                        k.op("dve", lambda e: e.tensor_tensor(out=tA[1][:, :], in0=x2[:, :], in1=sn[:, :], op=ALU.mult),
                             R=[PSB[1], B_sn], W=[B_tA[1]])
                        k.op("dve", lambda e: e.tensor_tensor(out=tA[2][:, :], in0=x1[:, :], in1=sn[:, :], op=ALU.mult),
                             R=[PSB[0], B_sn], W=[B_tA[2]])
                        k.op("dve", lambda e: e.tensor_tensor(out=tA[3][:, :], in0=x2[:, :], in1=cs[:, :], op=ALU.mult),
                             R=[PSB[1], B_cs], W=[B_tA[3]])
                        k.op("pool", lambda e, hl=hl, dst=dst: e.tensor_tensor(
                            out=dst[:, 2 * hl, :], in0=tA[0][:, :], in1=tA[1][:, :], op=ALU.subtract),
                            R=[B_tA[0], B_tA[1]], W=[B_dst])
                        k.op("pool", lambda e, hl=hl, dst=dst: e.tensor_tensor(
                            out=dst[:, 2 * hl + 1, :], in0=tA[2][:, :], in1=tA[3][:, :], op=ALU.add),
                            R=[B_tA[2], B_tA[3]], W=[B_dst])
                for tb in range(4):
                    for ch in range(4):
                        k.op("pe", lambda e, tb=tb, ch=ch: e.transpose(
                            out=pst[:, ch * 128:(ch + 1) * 128], in_=kTt[:, ch, tb * 128:(tb + 1) * 128], identity=IDB),
                            R=[B_kT, B_cb], W=[PSB[7]])
                    for hl in range(2):
                        k.op("dve", lambda e, tb=tb, hl=hl: e.tensor_scalar(
                            out=ktok[:, tb, hl * 256:(hl + 1) * 256], in0=pst[:, hl * 256:(hl + 1) * 256],
                            scalar1=cf[:, F_GC + hl:F_GC + hl + 1], scalar2=None, op0=ALU.mult),
                            R=[PSB[7], B_cf], W=[B_ktok])
                for tb in range(4):
                    for hl in range(2):
                        bank = 2 + pp % 2
                        pp += 1
                        for kc in range(8):
                            k.op("pe", lambda e, kc=kc, tb=tb, hl=hl, bank=bank: e.matmul(
                                ps[bank][:, :], lhsT=hb[:, kc, tb * 128:(tb + 1) * 128],
                                rhs=Win[:, kc, 1024 + hl * 512:1024 + (hl + 1) * 512],
                                start=(kc == 0), stop=(kc == 7)), R=[B_Win, B_hb], W=[PSB[bank]])
                        k.op("dve", lambda e, tb=tb, hl=hl, bank=bank: e.tensor_scalar(
                            out=w[:, tb, hl, :], in0=ps[bank][:, :], scalar1=cf[:, F_SCW + hl:F_SCW + hl + 1],
                            scalar2=None, op0=ALU.mult), R=[PSB[bank], B_cf], W=[B_w])
                for tb in range(4):
                    for hl in range(2):
                        bank = 2 + pp % 2
                        pp += 1
                        for kc in range(8):
                            k.op("pe", lambda e, kc=kc, tb=tb, hl=hl, bank=bank: e.matmul(
                                ps[bank][:, :], lhsT=hb[:, kc, tb * 128:(tb + 1) * 128],
                                rhs=Win[:, kc, 2048 + hl * 512:2048 + (hl + 1) * 512],
                                start=(kc == 0), stop=(kc == 7)), R=[B_Win, B_hb], W=[PSB[bank]])
                        ti = pp % 2
                        k.op("act", lambda e, bank=bank, ti=ti: e.activation(out=tA[ti][:, :], in_=ps[bank][:, :], func=AF.Exp, scale=-1.0),
                             R=[PSB[bank]], W=[B_tA[ti]])
                        k.op("act", lambda e, ti=ti: e.activation(out=tA[ti][:, :], in_=tA[ti][:, :], func=AF.Ln, bias=1.0),
                             R=[B_tA[ti]], W=[B_tA[ti]])
                        k.op("act", lambda e, ti=ti: e.activation(out=tA[ti][:, :], in_=tA[ti][:, :], func=AF.Exp, scale=-1.0),
                             R=[B_tA[ti]], W=[B_tA[ti]])
                        k.op("dve", lambda e, tb=tb, hl=hl, bank=bank, ti=ti: e.tensor_tensor(
                            out=sg[:, tb, hl, :], in0=ps[bank][:, :], in1=tA[ti][:, :], op=ALU.mult),
                            R=[PSB[bank], B_tA[ti]], W=[B_sg])
                for tb in range(4):
                    tsl = slice(tb * 128, (tb + 1) * 128)
                    for hl in range(2):
                        psS, B_psS = (ps[4], PSB[4]) if hl == 0 else (ps[2], PSB[2])
                        psP, B_psP = (ps[5], PSB[5]) if hl == 0 else (ps[3], PSB[3])
                        psU, B_psU = (ps[6], ps[1]), (PSB[6], PSB[1])
                        for hf in range(2):
                            k.op("pe", lambda e, hl=hl, hf=hf: e.matmul(
                                psS[:, 0:128], lhsT=kTt[:, 2 * hl + hf, tsl], rhs=qT[:, 2 * hl + hf, tsl],
                                start=(hf == 0), stop=(hf == 1)), R=[B_kT, B_qT], W=[B_psS])
                        k.op("dve", lambda e: e.tensor_tensor(out=sT[hl][:, :], in0=psS[:, 0:128], in1=MLE, op=ALU.mult),
                             R=[B_psS, B_cb], W=[B_sT[hl]])
                        for hf in range(2):
                            k.op("pe", lambda e, hl=hl, hf=hf: e.matmul(
                                psP[:, :], lhsT=qT[:, 2 * hl + hf, tsl], rhs=stb[:, hl, hf, :],
                                start=(hf == 0), stop=False), R=[B_qT, B_stb[hl][hf]], W=[B_psP])
                        k.op("pe", lambda e, hl=hl, tb=tb: e.matmul(
                            psP[:, :], lhsT=sT[hl][:, :], rhs=w[:, tb, hl, :], start=False, stop=True),
                            R=[B_sT[hl], B_w], W=[B_psP])
                        for hf in range(2):
                            k.op("pe", lambda e, hl=hl, hf=hf, tb=tb: e.matmul(
                                psU[hf][:, :], lhsT=ktok[:, tb, hl * 256 + hf * 128:hl * 256 + (hf + 1) * 128],
                                rhs=w[:, tb, hl, :], start=True, stop=True), R=[B_ktok, B_w], W=[B_psU[hf]])
                            k.op("dve", lambda e, hl=hl, hf=hf: e.scalar_tensor_tensor(
                                out=stf[:, hl, hf, :], in0=stf[:, hl, hf, :], scalar=cf[:, F_GC + hl:F_GC + hl + 1],
                                in1=psU[hf][:, :], op0=ALU.mult, op1=ALU.add),
                                R=[B_psU[hf], B_stf[hl][hf], B_cf], W=[B_stf[hl][hf]])
                            k.op("act", lambda e, hl=hl, hf=hf: e.activation(out=stb[:, hl, hf, :], in_=stf[:, hl, hf, :], func=AF.Copy),
                                 R=[B_stf[hl][hf]], W=[B_stb[hl][hf]])
                        k.op("dve", lambda e: e.bn_stats(out=st6[hl][:, :], in_=psP[:, :]), R=[B_psP], W=[B_st6[hl]])
                        k.op("dve", lambda e: e.bn_aggr(out=mv[hl][:, :], in_=st6[hl][:, :]), R=[B_st6[hl]], W=[B_mv[hl]])
                        k.op("dve", lambda e, hl=hl: e.tensor_tensor(
                            out=rs[hl][:, 0:1], in0=mv[hl][:, 1:2], in1=cf[:, F_EPS + hl:F_EPS + hl + 1], op=ALU.add),
                            R=[B_mv[hl], B_cf], W=[B_rs[hl]])
                        k.op("act", lambda e: e.activation(out=rs[hl][:, 1:2], in_=rs[hl][:, 0:1], func=AF.Ln), R=[B_rs[hl]], W=[B_rs[hl]])
                        k.op("act", lambda e: e.activation(out=rs[hl][:, 0:1], in_=rs[hl][:, 1:2], func=AF.Exp, scale=-0.5), R=[B_rs[hl]], W=[B_rs[hl]])
                        k.op("dve", lambda e: e.tensor_scalar(
                            out=on[hl][:, :], in0=psP[:, :], scalar1=mv[hl][:, 0:1], scalar2=rs[hl][:, 0:1],
                            op0=ALU.subtract, op1=ALU.mult), R=[B_psP, B_mv[hl], B_rs[hl]], W=[B_on[hl]])
                        k.op("pool", lambda e, hl=hl, tb=tb: e.tensor_tensor(
                            out=ytk[:, hl * 512:(hl + 1) * 512], in0=on[hl][:, :], in1=sg[:, tb, hl, :], op=ALU.mult),
                            R=[B_on[hl], B_sg], W=[B_ytk])
                    for fc in range(8):
                        k.op("pe", lambda e, fc=fc: e.transpose(
                            out=pst[:, fc * 128:(fc + 1) * 128], in_=ytk[:, fc * 128:(fc + 1) * 128], identity=IDB),
                            R=[B_ytk, B_cb], W=[PSB[7]])
                    k.op("act", lambda e, tb=tb: e.activation(
                        out=yT[:, :, tb * 128:(tb + 1) * 128],
                        in_=pst[:, :].rearrange("p (c t) -> p c t", c=8), func=AF.Copy),
                        R=[PSB[7]], W=[B_yT])
                for oc in range(8):
                    bank = pp % 2
                    pp += 1
                    for fc in range(8):
                        k.op("pe", lambda e, oc=oc, fc=fc, bank=bank: e.matmul(
                            ps[bank][:, :], lhsT=Wo[:, fc, oc * 128:(oc + 1) * 128], rhs=yT[:, fc, :],
                            start=(fc == 0), stop=(fc == 7)), R=[B_Wo, B_yT], W=[PSB[bank]])
                    k.op("act", lambda e, oc=oc, bank=bank: e.activation(out=stage[:, oc, :], in_=ps[bank][:, :], func=AF.Copy),
                         R=[PSB[bank]], W=[B_stage])
                store_partial(T, stage, B_stage)
            reduce_scatter_all()
            k.barrier()

    def phase_mlp(layer):
        last = layer == DEPTH - 1
        with contextlib.ExitStack() as es:
            def sb(name, shape, dt):
                return es.enter_context(nc.sbuf_tensor(un("ml_" + name), shape, dt))
            h = sb("h", [128, 8, TT], F32)
            dl = sb("d", [128, 8, TT], F32)
            hn = sb("hn", [128, 8, TT], BF16)
            sq = sb("sq", [128, 8, TT], BF16)
            tmp = sb("tmp", [128, TT], F32)
            rstd = sb("rstd", [128, TT], F32)
            u = sb("u", [128, 32, TT], BF16)
            rl = [sb(f"rl{i}", [128, TT], F32) for i in range(2)]
            wup = [sb(f"wup{i}", [128, 8, 1024], BF16) for i in range(2)]
            wdn = [sb(f"wdn{i}", [128, 32, 256], BF16) for i in range(2)]
            ps = [es.enter_context(nc.psum_tensor(un(f"ml_ps{i}"), [128, TT], F32)) for i in range(7)]
            B_h, B_d, B_hn, B_sq, B_tmp, B_rstd, B_u = (Buf(n) for n in ("mh", "md", "mhn", "msq", "mtmp", "mrstd", "mu"))
            B_rl = [Buf("rl0"), Buf("rl1")]
            B_wup = [Buf("wup0"), Buf("wup1")]
            B_wdn = [Buf("wdn0"), Buf("wdn1")]
            wi = 0
            wj = 0
            pp = 0
            for j in range(NOWN):
                k.dma("sp", h[:, :, :], hres_d[j], R=[B_hres[j]], W=[B_h])
                for half in range(2):
                    m = 2 * j + half
                    k.dma("sp", dl[:, :, half * 256:(half + 1) * 256],
                          rb_d[m].ap().rearrange("(c p) t -> p c t", p=128), R=[B_rb[m]], W=[B_d], nowaw=True)
                k.op("pool", lambda e: e.tensor_tensor(out=h[:, :, :], in0=h[:, :, :], in1=dl[:, :, :], op=ALU.add),
                     R=[B_h, B_d], W=[B_h])
                rmsnorm(h, B_h, 4 + layer, hn, B_hn, sq, B_sq, tmp, B_tmp, rstd, B_rstd, ps[6], PSB[6])
                for q in range(4):
                    wb, B_wb = wup[wi % 2], B_wup[wi % 2]
                    wi += 1
                    load_w(wb, wup_d[layer][:, q * 1024:(q + 1) * 1024], 8, B_wb)
                    for hcl in range(8):
                        hc = q * 8 + hcl
                        bank = pp % 2
                        pp += 1
                        for kc in range(8):
                            k.op("pe", lambda e, kc=kc, hcl=hcl, bank=bank, wb=wb: e.matmul(
                                ps[bank][:, :], lhsT=wb[:, kc, hcl * 128:(hcl + 1) * 128], rhs=hn[:, kc, :],
                                start=(kc == 0), stop=(kc == 7)), R=[B_wb, B_hn], W=[PSB[bank]])
                        k.op("act", lambda e, bank=bank: e.activation(out=rl[bank][:, :], in_=ps[bank][:, :], func=AF.Relu),
                             R=[PSB[bank]], W=[B_rl[bank]])
                        k.op("dve", lambda e, bank=bank, hc=hc: e.tensor_tensor(
                            out=u[:, hc, :], in0=rl[bank][:, :], in1=rl[bank][:, :], op=ALU.mult),
                            R=[B_rl[bank]], W=[B_u])
                for o in range(4):
                    wb, B_wb = wdn[wj % 2], B_wdn[wj % 2]
                    wj += 1
                    load_w(wb, wdn_d[layer][:, o * 256:(o + 1) * 256], 32, B_wb)
                    for ol in range(2):
                        oc = o * 2 + ol
                        bank = 2 + pp % 2
                        pp += 1
                        for hc in range(32):
                            k.op("pe", lambda e, hc=hc, ol=ol, bank=bank, wb=wb: e.matmul(
                                ps[bank][:, :], lhsT=wb[:, hc, ol * 128:(ol + 1) * 128], rhs=u[:, hc, :],
                                start=(hc == 0), stop=(hc == 31)), R=[B_wb, B_u], W=[PSB[bank]])
                        k.op("dve", lambda e, oc=oc, bank=bank: e.tensor_tensor(
                            out=h[:, oc, :], in0=h[:, oc, :], in1=ps[bank][:, :], op=ALU.add),
                            R=[PSB[bank], B_h], W=[B_h])
                if not last:
                    k.dma("sp", hres_d[j], h[:, :, :], R=[B_h], W=[B_hres[j]])
                    rmsnorm(h, B_h, layer + 1, hn, B_hn, sq, B_sq, tmp, B_tmp, rstd, B_rstd, ps[6], PSB[6])
                    emit_hn(j, hn, B_hn)
                else:
                    rmsnorm(h, B_h, 8, dl, B_d, sq, B_sq, tmp, B_tmp, rstd, B_rstd, ps[6], PSB[6])
                    for tb in range(4):
                        for half in range(2):
                            bank = 4 + half
                            for cl in range(4):
                                c = half * 4 + cl
                                k.op("pe", lambda e, tb=tb, c=c, cl=cl, bank=bank: e.transpose(
                                    out=ps[bank][:, cl * 128:(cl + 1) * 128], in_=dl[:, c, tb * 128:(tb + 1) * 128],
                                    identity=IDF), R=[B_d, B_cf], W=[PSB[bank]])
                            k.op("act", lambda e, tb=tb, half=half, bank=bank: e.activation(
                                out=h[:, tb * 2 + half, :], in_=ps[bank][:, :], func=AF.Copy),
                                R=[PSB[bank]], W=[B_h])
                    k.dma("sp", y_d[j * TT:(j + 1) * TT, :].rearrange("(tb p) f -> p tb f", p=128),
                          h[:, :, :].rearrange("p (tb x) f -> p tb (x f)", x=2), R=[B_h], W=[B_y])
            k.barrier()

    B_y = Buf("yout")
    phases = [("p0", phase0)]
    for layer in range(DEPTH):
        if layer % 2 == 0:
            phases.append((f"mix{layer}", lambda layer=layer: phase_sb(layer // 2)))
        else:
            phases.append((f"mix{layer}", lambda layer=layer: phase_ret(layer // 2)))
        phases.append((f"mlp{layer}", lambda layer=layer: phase_mlp(layer)))
    for name, fn in phases:
        fn()
        if stop_after == name:
            break
    if debug:
        dbg_h = nc.dram_tensor("dbg_h", [NOWN, 128, 8, TT], F32, kind="ExternalOutput")
        dbg_yb = nc.dram_tensor("dbg_yb", [NOWN, 2 * D, TT], BF16, kind="ExternalOutput")
        dbg_rb = nc.dram_tensor("dbg_rb", [8, 8 * 128, 256], F32, kind="ExternalOutput")
        dbg_pb = nc.dram_tensor("dbg_pb", [8, 16 * 128, 256], F32, kind="ExternalOutput")
        B_dbg = Buf("dbg")
        k.barrier(engines=("sp",))
        for j in range(NOWN):
            k.dma("sp", dbg_h[j], hres_d[j], W=[B_dbg], nowaw=True)
            k.dma("sp", dbg_yb[j], yb_d[j].ap(), W=[B_dbg], nowaw=True)
        for m in range(8):
            k.dma("sp", dbg_rb[m], rb_d[m].ap(), W=[B_dbg], nowaw=True)
            k.dma("sp", dbg_pb[m], pb_d[m].ap(), W=[B_dbg], nowaw=True)
    k.barrier(engines=("sp",))
    return nc


def _consts(p):
    j = np.arange(128)
    cbm = np.zeros((128, NCB), np.float32)
    cbm[:, C_TRI:C_TRI + 128] = (j[:, None] >= j[None, :])
    cbm[:, C_ONES:C_ONES + 128] = 1.0
    cbm[:, C_MLE:C_MLE + 128] = (j[:, None] <= j[None, :])
    cbm[:, C_ID:C_ID + 128] = np.eye(128)
    t = np.arange(512)
    for d in range(4):
        cbm[:, C_MASK + d * 512:C_MASK + (d + 1) * 512] = (j[:, None] + 128 * d < t[None, :])
    cbm[:, C_UTRI:C_UTRI + 128] = (j[:, None] < j[None, :])
    return cbm


def _cf(p, g_mix, g_mlp, g_final):
    cfm = np.zeros((128, NCF), np.float32)
    cfm[:, F_ID:F_ID + 128] = np.eye(128)
    gains = np.concatenate([g_mix, g_mlp, g_final[None, :]], axis=0)
    cfm[:, F_G:F_G + 72] = gains.reshape(9, 8, 128).transpose(2, 0, 1).reshape(128, 72)
    j = np.arange(128, dtype=np.float64)
    for hl in range(2):
        hd = 2 * p + hl
        lg = np.log1p(-np.exp2(-5.0 - hd))
        cfm[:, F_SCW + hl] = np.exp(lg * (-1.0 - j)) / 16.0
        cfm[:, F_GC + hl] = np.exp(lg * 128.0)
        cfm[:, F_EPS + hl] = GN_EPS * np.exp(lg * (-2.0 * (j + 1.0)))
    return cfm


def _rope_tables():
    half = 128
    inv_freq = (1.0 / (10000.0 ** np.linspace(0.0, 1.0, half, dtype=np.float32))).astype(np.float32)
    pos = np.arange(S, dtype=np.float32)
    ang = (inv_freq[:, None] * pos[None, :]).astype(np.float32).astype(np.float64)
    return np.cos(ang).astype(np.float32), np.sin(ang).astype(np.float32)


_NC_CACHE = {}


def make_in_maps(x, w_sb_in, w_sb_out, w_ret_in, w_ret_out, g_mix, g_mlp, w_mlp_up, w_mlp_down, g_final):
    f = lambda a: np.ascontiguousarray(np.asarray(a, dtype=np.float32))
    x, w_sb_in, w_sb_out, w_ret_in, w_ret_out = f(x), f(w_sb_in), f(w_sb_out), f(w_ret_in), f(w_ret_out)
    g_mix, g_mlp, w_mlp_up, w_mlp_down, g_final = f(g_mix), f(g_mlp), f(w_mlp_up), f(w_mlp_down), f(g_final)
    cosT, sinT = _rope_tables()
    in_maps = []
    for c in range(8):
        b, p = c // 2, c % 2
        sl = slice(512 * p, 512 * p + 512)
        wsi = np.concatenate([w_sb_in[:, :, 0:1024][:, :, sl], w_sb_in[:, :, 1024:2048][:, :, sl],
                              w_sb_in[:, :, 2048:3072][:, :, sl]], axis=2)
        wso = w_sb_out[:, sl, :]
        sl2 = slice(1024 * p, 1024 * p + 1024)
        wri = np.concatenate([w_ret_in[:, :, 0:1024][:, :, sl], w_ret_in[:, :, 1024:2048][:, :, sl],
                              w_ret_in[:, :, 2048:4096][:, :, sl2], w_ret_in[:, :, 4096:6144][:, :, sl2]], axis=2)
        wro = w_ret_out[:, sl2, :]
        in_maps.append({
            "x": f(x[b, p * OWN:(p + 1) * OWN, :]),
            "w_sb_in": f(wsi), "w_sb_out": f(wso), "w_ret_in": f(wri), "w_ret_out": f(wro),
            "w_up": w_mlp_up, "w_dn": w_mlp_down,
            "cb": _consts(p), "cf": _cf(p, g_mix, g_mlp, g_final),
            "cosT": cosT, "sinT": sinT,
        })
    return in_maps


def kernel(x, w_sb_in, w_sb_out, w_ret_in, w_ret_out, g_mix, g_mlp, w_mlp_up, w_mlp_down, g_final):
    if "nc" not in _NC_CACHE:
        _NC_CACHE["nc"] = build_program()
    nc = _NC_CACHE["nc"]
    in_maps = make_in_maps(x, w_sb_in, w_sb_out, w_ret_in, w_ret_out, g_mix, g_mlp, w_mlp_up, w_mlp_down, g_final)
    res = run_bass_kernel_spmd(nc, in_maps, core_ids=list(range(8)))
    out = np.empty((NB, S, D), np.float32)
    for c in range(8):
        b, p = c // 2, c % 2
        out[b, p * OWN:(p + 1) * OWN, :] = np.asarray(res.results[c]["y"], dtype=np.float32)
    return out
```

```python
import contextlib
import numpy as np
import concourse.bass as bass
import concourse.mybir as mybir
from concourse.bass_utils import run_bass_kernel_spmd

F32 = mybir.dt.float32
BF16 = mybir.dt.bfloat16
AF = mybir.ActivationFunctionType
ALU = mybir.AluOpType

D = 1024
S = 4096
NB = 4
DEPTH = 4
OWN = 2048
TT = 512
NOWN = OWN // TT
NALL = S // TT
HID = 4096
GROUPS = [[0, 1], [2, 3], [4, 5], [6, 7]]
RMS_EPS = 1e-6
SB_TMAX = 8
SB_ATT = True
SB_RS = True
SB_LVL = 9
GN_EPS = 1e-6

C_TRI, C_ONES, C_MLE, C_ID, C_MASK = 0, 128, 256, 384, 512
C_UTRI = 512 + 4 * 512
NCB = C_UTRI + 128
F_ID, F_G, F_SCW, F_GC, F_EPS = 0, 128, 200, 202, 204
NCF = 206


class Buf:
    __slots__ = ("name", "w", "r", "dsem", "dcnt", "key", "excl")

    def __init__(self, name, excl=False):
        self.name = name
        self.excl = excl
        self.w = None
        self.r = {}
        self.dsem = None
        self.dcnt = 0


class KB:
    def __init__(self, nc):
        self.nc = nc
        self.E = {"pe": nc.tensor, "act": nc.scalar, "dve": nc.vector, "pool": nc.gpsimd, "sp": nc.sync}
        self.sem = {e: nc.alloc_semaphore("c_" + e) for e in ("pe", "act", "dve", "pool")}
        self.cnt = {e: 0 for e in self.sem}
        self.seen = {e: {} for e in self.E}
        self.latest = {}
        self.nsem = 4

    def _deps(self, R, W, nowaw=False):
        toks = []
        for b in R:
            if b.w is not None:
                toks.append(b.w)
            if b.excl:
                toks.extend(b.r.values())
        for b in W:
            if b.w is not None and not (nowaw and b.w[3] == "dma"):
                toks.append(b.w)
            toks.extend(b.r.values())
        return toks

    def _wait(self, e, toks):
        for (key, sem, val, prod) in toks:
            if prod == "pe" and e == "pe":
                continue
            if self.seen[e].get(key, 0) < val:
                self.E[e].wait_ge(sem, val)
                self.seen[e][key] = val

    def _commit(self, tok, R, W):
        self.latest[tok[0]] = tok
        for b in R:
            b.r[tok[0]] = tok
        for b in W:
            b.w = tok
            b.r = {}

    def op(self, e, fn, R=(), W=()):
        self._wait(e, self._deps(R, W))
        inst = fn(self.E[e])
        self.cnt[e] += 1
        inst.then_inc(self.sem[e], 1)
        self._commit(("c_" + e, self.sem[e], self.cnt[e], e), R, W)

    def _dsem(self, dst):
        if dst.dsem is None:
            self.nsem += 1
            dst.key = "d%d_%s" % (self.nsem, dst.name)
            dst.dsem = self.nc.alloc_semaphore(dst.key)
        return dst.dsem

    def dma(self, q, out, in_, R=(), W=(), nowaw=False):
        dst = W[0]
        self._wait(q, self._deps(R, W, nowaw))
        sem = self._dsem(dst)
        inst = self.E[q].dma_start(out=out, in_=in_)
        dst.dcnt += 16
        inst.then_inc(sem, 16)
        self._commit((dst.key, sem, dst.dcnt, "dma"), R, W)

    def cc(self, kind, alu, in_ap, out_ap, R=(), W=()):
        dst = W[0]
        self._wait("pool", self._deps(R, W))
        sem = self._dsem(dst)
        inst = self.nc.gpsimd.collective_compute(kind, alu, replica_groups=GROUPS, ins=[in_ap], outs=[out_ap])
        dst.dcnt += 1
        inst.then_inc(sem)
        self._commit((dst.key, sem, dst.dcnt, "cc"), R, W)

    def barrier(self, engines=("pe", "act", "dve", "pool", "sp")):
        toks = [t for t in self.latest.values() if t[3] != "cc" or len(engines) == 1]
        for e in engines:
            self._wait(e, toks)


def build_program(stop_after=None, debug=False):
    nc = bass.Bass("TRN2", target_bir_lowering=False)
    k = KB(nc)

    _uid = [0]

    def un(name):
        _uid[0] += 1
        return "%s_%d" % (name, _uid[0])

    def dram_in(name, shape, dt=F32):
        return nc.dram_tensor(name, shape, dt, kind="ExternalInput")

    x_d = dram_in("x", [OWN, D])
    wsi_d = dram_in("w_sb_in", [2, D, 1536])
    wso_d = dram_in("w_sb_out", [2, 512, D])
    wri_d = dram_in("w_ret_in", [2, D, 3072])
    wro_d = dram_in("w_ret_out", [2, 1024, D])
    wup_d = dram_in("w_up", [DEPTH, D, HID])
    wdn_d = dram_in("w_dn", [DEPTH, HID, D])
    cb_d = dram_in("cb", [128, NCB])
    cf_d = dram_in("cf", [128, NCF])
    cos_d = dram_in("cosT", [128, S])
    sin_d = dram_in("sinT", [128, S])
    y_d = nc.dram_tensor("y", [OWN, D], F32, kind="ExternalOutput")

    hres_d = nc.dram_tensor("hres", [NOWN, 128, 8, TT], F32)
    xb_d = [nc.dram_tensor(f"xb{j}", [D, TT], BF16) for j in range(NOWN)]
    yb_d = [nc.dram_tensor(f"yb{j}", [2 * D, TT], BF16) for j in range(NOWN)]
    pb_d = [nc.dram_tensor(f"pb{m}", [2 * 8 * 128, 256], F32) for m in range(8)]
    rb_d = [nc.dram_tensor(f"rb{m}", [8 * 128, 256], F32) for m in range(8)]
    B_hres = [Buf(f"hres{j}") for j in range(NOWN)]
    B_xb = [Buf(f"xb{j}") for j in range(NOWN)]
    B_yb = [Buf(f"yb{j}") for j in range(NOWN)]
    B_pb = [Buf(f"pb{m}") for m in range(8)]
    B_rb = [Buf(f"rb{m}") for m in range(8)]

    cb = nc.alloc_sbuf_tensor("cb_s", [128, NCB], BF16)
    cf = nc.alloc_sbuf_tensor("cf_s", [128, NCF], F32)
    B_cb, B_cf = Buf("cb"), Buf("cf")
    k.dma("pool", cb[:, :], cb_d[:, :], W=[B_cb])
    k.dma("sp", cf[:, :], cf_d[:, :], W=[B_cf])
    TRI = cb[:, C_TRI:C_TRI + 128]
    ONES = cb[:, C_ONES:C_ONES + 128]
    UTRI = cb[:, C_UTRI:C_UTRI + 128]
    MLE = cb[:, C_MLE:C_MLE + 128]
    IDB = cb[:, C_ID:C_ID + 128]
    IDF = cf[:, F_ID:F_ID + 128]

    def load_w(dst, src2d, nk, B_dst):
        for kc in range(nk):
            k.dma("pool", dst[:, kc, :], src2d[kc * 128:(kc + 1) * 128, :], W=[B_dst], nowaw=True)

    def maskd(d):
        return cb[:, C_MASK + d * 512:C_MASK + (d + 1) * 512]

    PSB = [Buf(f"ps{i}", excl=True) for i in range(8)]

    def rmsnorm(h, B_h, gidx, out, B_out, sq, B_sq, tmp, B_tmp, rstd, B_rstd, ps, B_ps):
        k.op("act", lambda e: e.activation(out=sq[:, :, :], in_=h[:, :, :], func=AF.Square),
             R=[B_h], W=[B_sq])
        for c in range(8):
            k.op("pe", lambda e, c=c: e.matmul(ps[:, :], lhsT=ONES, rhs=sq[:, c, :], start=(c == 0), stop=(c == 7)),
                 R=[B_sq, B_cb], W=[B_ps])
        k.op("act", lambda e: e.activation(out=tmp[:, :], in_=ps[:, :], func=AF.Ln, scale=1.0 / D, bias=RMS_EPS),
             R=[B_ps], W=[B_tmp])
        k.op("act", lambda e: e.activation(out=rstd[:, :], in_=tmp[:, :], func=AF.Exp, scale=-0.5),
             R=[B_tmp], W=[B_rstd])
        for c in range(8):
            k.op("dve", lambda e, c=c: e.scalar_tensor_tensor(
                out=out[:, c, :], in0=h[:, c, :], scalar=cf[:, F_G + gidx * 8 + c:F_G + gidx * 8 + c + 1],
                in1=rstd[:, :], op0=ALU.mult, op1=ALU.mult), R=[B_h, B_rstd, B_cf], W=[B_out])

    def emit_hn(j, hn, B_hn):
        k.dma("sp", xb_d[j].ap().rearrange("(c p) t -> p c t", p=128), hn[:, :, :], R=[B_hn], W=[B_xb[j]])
        k.cc("AllGather", ALU.bypass, xb_d[j].ap(), yb_d[j].ap(), R=[B_xb[j]], W=[B_yb[j]])

    def load_hn(T, hn, B_hn):
        j, r = T % NOWN, T // NOWN
        k.dma("sp", hn[:, :, :], yb_d[j][r * D:(r + 1) * D, :].rearrange("(c p) t -> p c t", p=128),
              R=[B_yb[j]], W=[B_hn])

    def store_partial(T, stage, B_stage):
        j, r = T % NOWN, T // NOWN
        for half in range(2):
            m = 2 * j + half
            k.dma("sp", pb_d[m][r * 1024:(r + 1) * 1024, :].rearrange("(c p) t -> p c t", p=128),
                  stage[:, :, half * 256:(half + 1) * 256], R=[B_stage], W=[B_pb[m]], nowaw=True)

    def reduce_scatter_tile(T):
        if T >= NOWN:
            for m in (2 * (T - NOWN), 2 * (T - NOWN) + 1):
                k.cc("ReduceScatter", ALU.add, pb_d[m].ap(), rb_d[m].ap(), R=[B_pb[m]], W=[B_rb[m]])

    def phase0():
        with contextlib.ExitStack() as es:
            X = es.enter_context(nc.sbuf_tensor("p0_X", [128, 4, D], F32))
            h = es.enter_context(nc.sbuf_tensor("p0_h", [128, 8, TT], F32))
            hn = es.enter_context(nc.sbuf_tensor("p0_hn", [128, 8, TT], BF16))
            sq = es.enter_context(nc.sbuf_tensor("p0_sq", [128, 8, TT], BF16))
            tmp = es.enter_context(nc.sbuf_tensor("p0_tmp", [128, TT], F32))
            rstd = es.enter_context(nc.sbuf_tensor("p0_rstd", [128, TT], F32))
            ps = [es.enter_context(nc.psum_tensor(f"p0_ps{i}", [128, TT], F32)) for i in range(3)]
            B_X, B_h, B_hn, B_sq, B_tmp, B_rstd = (Buf(n) for n in ("p0X", "p0h", "p0hn", "p0sq", "p0tmp", "p0rstd"))
            for j in range(NOWN):
                k.dma("sp", X[:, :, :], x_d[j * TT:(j + 1) * TT, :].rearrange("(tb p) f -> p tb f", p=128), W=[B_X])
                for c in range(8):
                    pb_ = ps[c % 2]
                    for tb in range(4):
                        k.op("pe", lambda e, c=c, tb=tb, pb_=pb_: e.transpose(
                            out=pb_[:, tb * 128:(tb + 1) * 128], in_=X[:, tb, c * 128:(c + 1) * 128], identity=IDF),
                            R=[B_X, B_cf], W=[PSB[c % 2]])
                    k.op("act", lambda e, c=c, pb_=pb_: e.activation(out=h[:, c, :], in_=pb_[:, :], func=AF.Copy),
                         R=[PSB[c % 2]], W=[B_h])
                k.dma("sp", hres_d[j], h[:, :, :], R=[B_h], W=[B_hres[j]])
                rmsnorm(h, B_h, 0, hn, B_hn, sq, B_sq, tmp, B_tmp, rstd, B_rstd, ps[2], PSB[2])
                emit_hn(j, hn, B_hn)
            k.barrier()

    def phase_sb(li):
        with contextlib.ExitStack() as es:
            def sb(name, shape, dt):
                return es.enter_context(nc.sbuf_tensor(un("sb_" + name), shape, dt))
            Win = sb("Win", [128, 8, 1536], BF16)
            Wo = sb("Wo", [128, 4, D], BF16)
            kT = sb("kT", [128, 4, S], BF16)
            vA = sb("vA", [128, 32, 512], BF16)
            hn = [sb(f"hn{i}", [128, 8, TT], BF16) for i in range(2)]
            qT = sb("qT", [128, 4, TT], BF16)
            Ee = [sb(f"E{i}", [128, 2 * TT], F32) for i in range(3)]
            sp = [sb(f"sp{i}", [128, 2 * TT], BF16) for i in range(2)]
            Gg = sb("G", [128, 2 * TT], F32)
            accf = sb("accf", [128, 2 * TT], F32)
            accb = [sb(f"accb{i}", [128, 2 * TT], BF16) for i in range(2)]
            At = [sb(f"At{i}", [128, 2 * TT], BF16) for i in range(2)]
            oT = sb("oT", [128, 4, TT], BF16)
            stage = sb("stage", [128, 8, TT], F32)
            ps = [es.enter_context(nc.psum_tensor(un(f"sb_ps{i}"), [128, TT], F32)) for i in range(2)]
            Zab = es.enter_context(nc.psum_tensor(un("sb_Z"), [128, 2 * TT], F32))
            Cab = es.enter_context(nc.psum_tensor(un("sb_C"), [128, 2 * TT], F32))
            Oab = es.enter_context(nc.psum_tensor(un("sb_O"), [128, 2 * TT], F32))
            B_Win, B_Wo = Buf("sbWin"), Buf("sbWo")
            B_kT = [Buf(f"kT{i}") for i in range(NALL)]
            B_v = [Buf(f"v{i}") for i in range(NALL)]
            B_hn = [Buf("sbhn0"), Buf("sbhn1")]
            B_qT, B_nqT, B_oT, B_stage = Buf("qT"), Buf("nqT"), Buf("oT"), Buf("sbstage")
            B_E = [Buf("E0"), Buf("E1"), Buf("E2")]
            B_sp = [Buf("sp0"), Buf("sp1")]
            B_G = Buf("G")
            B_accf = Buf("accf")
            B_accb = [Buf("accb0"), Buf("accb1")]
            B_At = [Buf("At0"), Buf("At1")]

            load_w(Win, wsi_d[li], 8, B_Win)
            load_w(Wo, wso_d[li], 4, B_Wo)
            load_hn(0, hn[0], B_hn[0])
            pp = 0
            for T in range(SB_TMAX):
                hb, B_hb = hn[T % 2], B_hn[T % 2]
                if T + 1 < NALL:
                    load_hn(T + 1, hn[(T + 1) % 2], B_hn[(T + 1) % 2])
                for pr in range(4):
                    for which in range(2):
                        bank = pp % 2
                        pp += 1
                        col0 = which * 512 + pr * 128
                        for kc in range(8):
                            k.op("pe", lambda e, kc=kc, col0=col0, bank=bank: e.matmul(
                                ps[bank][:, :], lhsT=Win[:, kc, col0:col0 + 128], rhs=hb[:, kc, :],
                                start=(kc == 0), stop=(kc == 7)), R=[B_Win, B_hb], W=[PSB[bank]])
                        if which == 0:
                            k.op("dve", lambda e, pr=pr, bank=bank: e.tensor_copy(out=qT[:, pr, :], in_=ps[bank][:, :]),
                                 R=[PSB[bank]], W=[B_qT])
                        else:
                            k.op("act", lambda e, pr=pr, bank=bank: e.activation(
                                out=kT[:, pr, T * TT:(T + 1) * TT], in_=ps[bank][:, :], func=AF.Copy),
                                R=[PSB[bank]], W=[B_kT[T]])
                for tb in range(4 if SB_LVL >= 2 else 0):
                    bank = pp % 2
                    pp += 1
                    for kc in range(8):
                        k.op("pe", lambda e, kc=kc, tb=tb, bank=bank: e.matmul(
                            ps[bank][:, :], lhsT=hb[:, kc, tb * 128:(tb + 1) * 128], rhs=Win[:, kc, 1024:1536],
                            start=(kc == 0), stop=(kc == 7)), R=[B_Win, B_hb], W=[PSB[bank]])
                    k.op("dve", lambda e, tb=tb, bank=bank: e.tensor_copy(out=vA[:, T * 4 + tb, :], in_=ps[bank][:, :]),
                         R=[PSB[bank]], W=[B_v[T]])
                nst = 4 * T + 4
                BZ = (PSB[2], PSB[3]); BC = (PSB[4], PSB[5]); BO = (PSB[6], PSB[7])
                for pr in range(4 if SB_ATT else 0):
                    offs = (0, 64)

                    def hs(s_):
                        return slice(s_ * TT, (s_ + 1) * TT)

                    def zmm(i):
                        kb = 4 * T + 3 - i
                        for s_ in range(2):
                            o_ = offs[s_]
                            k.op("pe", lambda e: e.matmul(
                                Zab[:, hs(s_)], lhsT=kT[o_:o_ + 64, pr, kb * 128:(kb + 1) * 128],
                                rhs=qT[o_:o_ + 64, pr, :], start=True, stop=True),
                                R=[B_kT[kb // 4], B_qT], W=[BZ[s_]])

                    def act_E(i):
                        k.op("act", lambda e: e.activation(out=Ee[i % 3][:, :], in_=Zab[:, :], func=AF.Exp, scale=0.125),
                             R=[BZ[0], BZ[1]], W=[B_E[i % 3]])

                    def avmm(i):
                        kb = 4 * T + 3 - i
                        par = i % 2
                        for s_ in range(2):
                            k.op("pe", lambda e: e.matmul(
                                Oab[:, hs(s_)], lhsT=vA[:, kb, pr * 128:(pr + 1) * 128], rhs=At[par][:, hs(s_)],
                                start=(i == 0), stop=(i == nst - 1)), R=[B_v[kb // 4], B_At[par]], W=[BO[s_]])

                    zmm(0)
                    act_E(0)
                    for i in range(nst):
                        kb = 4 * T + 3 - i
                        dz = kb - 4 * T
                        par = i % 2
                        if i + 1 < nst:
                            zmm(i + 1)
                        k.op("act", lambda e: e.activation(out=sp[par][:, :], in_=Ee[i % 3][:, :], func=AF.Ln, bias=1.0),
                             R=[B_E[i % 3]], W=[B_sp[par]])
                        if dz >= 0:
                            for s_ in range(2):
                                k.op("pool", lambda e: e.tensor_tensor(
                                    out=sp[par][:, hs(s_)], in0=sp[par][:, hs(s_)], in1=maskd(dz), op=ALU.mult),
                                    R=[B_sp[par], B_cb], W=[B_sp[par]])
                        if i + 1 < nst:
                            act_E(i + 1)
                        for s_ in range(2):
                            k.op("pe", lambda e: e.matmul(Cab[:, hs(s_)], lhsT=TRI, rhs=sp[par][:, hs(s_)],
                                                          start=True, stop=(i == 0)),
                                 R=[B_sp[par], B_cb], W=[BC[s_]])
                            if i > 0:
                                k.op("pe", lambda e: e.matmul(Cab[:, hs(s_)], lhsT=ONES, rhs=accb[par][:, hs(s_)],
                                                              start=False, stop=True),
                                     R=[B_accb[par], B_cb], W=[BC[s_]])
                        k.op("act", lambda e: e.activation(out=Gg[:, :], in_=Cab[:, :], func=AF.Exp, scale=-1.0),
                             R=[BC[0], BC[1]], W=[B_G])
                        if i > 0:
                            avmm(i - 1)
                        if i + 1 < nst:
                            if i == 0:
                                k.op("dve", lambda e: e.tensor_copy(out=accf[:, :], in_=sp[par][:, :]),
                                     R=[B_sp[par]], W=[B_accf])
                            else:
                                k.op("dve", lambda e: e.tensor_tensor(out=accf[:, :], in0=accf[:, :], in1=sp[par][:, :], op=ALU.add),
                                     R=[B_sp[par], B_accf], W=[B_accf])
                            k.op("dve", lambda e: e.tensor_copy(out=accb[1 - par][:, :], in_=accf[:, :]),
                                 R=[B_accf], W=[B_accb[1 - par]])
                        k.op("dve", lambda e: e.tensor_tensor(out=At[par][:, :], in0=Ee[i % 3][:, :], in1=Gg[:, :], op=ALU.mult),
                             R=[B_E[i % 3], B_G], W=[B_At[par]])
                        if dz >= 0:
                            for s_ in range(2):
                                k.op("pool", lambda e: e.tensor_tensor(
                                    out=At[par][:, hs(s_)], in0=At[par][:, hs(s_)], in1=maskd(dz), op=ALU.mult),
                                    R=[B_At[par], B_cb], W=[B_At[par]])
                    avmm(nst - 1)
                    for s_ in range(2):
                        o_ = offs[s_]
                        k.op("dve", lambda e: e.tensor_copy(out=oT[o_:o_ + 64, pr, :], in_=Oab[o_:o_ + 64, hs(s_)]),
                             R=[BO[s_]], W=[B_oT])
                for oc in range(8 if SB_LVL >= 3 else 0):
                    bank = pp % 2
                    pp += 1
                    for pc in range(4):
                        k.op("pe", lambda e, oc=oc, pc=pc, bank=bank: e.matmul(
                            ps[bank][:, :], lhsT=Wo[:, pc, oc * 128:(oc + 1) * 128], rhs=oT[:, pc, :],
                            start=(pc == 0), stop=(pc == 3)), R=[B_Wo, B_oT], W=[PSB[bank]])
                    k.op("act", lambda e, oc=oc, bank=bank: e.activation(out=stage[:, oc, :], in_=ps[bank][:, :], func=AF.Copy),
                         R=[PSB[bank]], W=[B_stage])
                if SB_LVL >= 4:
                    store_partial(T, stage, B_stage)
                    if SB_RS:
                        reduce_scatter_tile(T)
            k.barrier()

    def phase_ret(li):
        with contextlib.ExitStack() as es:
            def sb(name, shape, dt):
                return es.enter_context(nc.sbuf_tensor(un("rt_" + name), shape, dt))
            Win = sb("Win", [128, 8, 3072], BF16)
            Wo = sb("Wo", [128, 8, D], BF16)
            hn = [sb(f"hn{i}", [128, 8, TT], BF16) for i in range(2)]
            csb = [sb(f"cos{i}", [128, TT], F32) for i in range(2)]
            snb = [sb(f"sin{i}", [128, TT], F32) for i in range(2)]
            qT = sb("qT", [128, 4, TT], BF16)
            kTt = sb("kT", [128, 4, TT], BF16)
            ktok = sb("ktok", [128, 4, 512], BF16)
            w = sb("w", [128, 4, 2, 512], BF16)
            sg = sb("sg", [128, 4, 2, 512], BF16)
            tA = [sb(f"tA{i}", [128, TT], F32) for i in range(4)]
            sT = [sb(f"sT{i}", [128, 128], BF16) for i in range(2)]
            stf = sb("stf", [128, 2, 2, 512], F32)
            stb = sb("stb", [128, 2, 2, 512], BF16)
            on = [sb(f"on{i}", [128, 512], F32) for i in range(2)]
            ytk = sb("ytk", [128, 1024], BF16)
            yT = sb("yT", [128, 8, TT], BF16)
            stage = sb("stage", [128, 8, TT], F32)
            st6 = [sb(f"st6{i}", [128, 6], F32) for i in range(2)]
            mv = [sb(f"mv{i}", [128, 2], F32) for i in range(2)]
            rs = [sb(f"rs{i}", [128, 2], F32) for i in range(2)]
            ps = [es.enter_context(nc.psum_tensor(un(f"rt_ps{i}"), [128, TT], F32)) for i in range(7)]
            pst = es.enter_context(nc.psum_tensor(un("rt_pst"), [128, 1024], BF16))
            B_Win, B_Wo = Buf("rtWin"), Buf("rtWo")
            B_hn = [Buf("rthn0"), Buf("rthn1")]
            B_qT, B_kT, B_ktok, B_w, B_sg = (Buf(n) for n in ("rqT", "rkT", "ktok", "w", "sg"))
            B_csb = [Buf("cs0"), Buf("cs1")]
            B_snb = [Buf("sn0"), Buf("sn1")]
            B_tA = [Buf(f"tA{i}") for i in range(4)]
            B_ytk, B_yT, B_stage = (Buf(n) for n in ("ytk", "yT", "rstage"))
            B_sT, B_on, B_st6, B_mv, B_rs = ([Buf(n + "0"), Buf(n + "1")] for n in ("sT", "on", "st6", "mv", "rs"))
            B_stf = [[Buf(f"stf{a}{b}") for b in range(2)] for a in range(2)]
            B_stb = [[Buf(f"stb{a}{b}") for b in range(2)] for a in range(2)]

            load_w(Win, wri_d[li], 8, B_Win)
            load_w(Wo, wro_d[li], 8, B_Wo)
            for hl in range(2):
                for hf in range(2):
                    k.op("dve", lambda e, hl=hl, hf=hf: e.memset(stf[:, hl, hf, :], 0.0), W=[B_stf[hl][hf]])
                    k.op("dve", lambda e, hl=hl, hf=hf: e.memset(stb[:, hl, hf, :], 0.0), W=[B_stb[hl][hf]])
            load_hn(0, hn[0], B_hn[0])
            pp = 0
            for T in range(NALL):
                hb, B_hb = hn[T % 2], B_hn[T % 2]
                if T + 1 < NALL:
                    load_hn(T + 1, hn[(T + 1) % 2], B_hn[(T + 1) % 2])
                if T == 0:
                    k.dma("sp", csb[0][:, :], cos_d[:, 0:TT], W=[B_csb[0]])
                    k.dma("sp", snb[0][:, :], sin_d[:, 0:TT], W=[B_snb[0]])
                if T + 1 < NALL:
                    k.dma("sp", csb[(T + 1) % 2][:, :], cos_d[:, (T + 1) * TT:(T + 2) * TT], W=[B_csb[(T + 1) % 2]])
                    k.dma("sp", snb[(T + 1) % 2][:, :], sin_d[:, (T + 1) * TT:(T + 2) * TT], W=[B_snb[(T + 1) % 2]])
                cs, sn, B_cs, B_sn = csb[T % 2], snb[T % 2], B_csb[T % 2], B_snb[T % 2]
                for which in range(2):
                    dst, B_dst = (qT, B_qT) if which == 0 else (kTt, B_kT)
                    for hl in range(2):
                        for hf in range(2):
                            col0 = which * 512 + hl * 256 + hf * 128
                            for kc in range(8):
                                k.op("pe", lambda e, kc=kc, col0=col0, hf=hf: e.matmul(
                                    ps[hf][:, :], lhsT=Win[:, kc, col0:col0 + 128], rhs=hb[:, kc, :],
                                    start=(kc == 0), stop=(kc == 7)), R=[B_Win, B_hb], W=[PSB[hf]])
                        x1, x2 = ps[0], ps[1]
                        k.op("dve", lambda e: e.tensor_tensor(out=tA[0][:, :], in0=x1[:, :], in1=cs[:, :], op=ALU.mult),
                             R=[PSB[0], B_cs], W=[B_tA[0]])
                        k.op("dve", lambda e: e.tensor_tensor(out=tA[1][:, :], in0=x2[:, :], in1=sn[:, :], op=ALU.mult),
                             R=[PSB[1], B_sn], W=[B_tA[1]])
                        k.op("dve", lambda e: e.tensor_tensor(out=tA[2][:, :], in0=x1[:, :], in1=sn[:, :], op=ALU.mult),
                             R=[PSB[0], B_sn], W=[B_tA[2]])
                        k.op("dve", lambda e: e.tensor_tensor(out=tA[3][:, :], in0=x2[:, :], in1=cs[:, :], op=ALU.mult),
                             R=[PSB[1], B_cs], W=[B_tA[3]])
                        k.op("pool", lambda e, hl=hl, dst=dst: e.tensor_tensor(
                            out=dst[:, 2 * hl, :], in0=tA[0][:, :], in1=tA[1][:, :], op=ALU.subtract),
                            R=[B_tA[0], B_tA[1]], W=[B_dst])
                        k.op("pool", lambda e, hl=hl, dst=dst: e.tensor_tensor(
                            out=dst[:, 2 * hl + 1, :], in0=tA[2][:, :], in1=tA[3][:, :], op=ALU.add),
                            R=[B_tA[2], B_tA[3]], W=[B_dst])
                for tb in range(4):
                    for ch in range(4):
                        k.op("pe", lambda e, tb=tb, ch=ch: e.transpose(
                            out=pst[:, ch * 128:(ch + 1) * 128], in_=kTt[:, ch, tb * 128:(tb + 1) * 128], identity=IDB),
                            R=[B_kT, B_cb], W=[PSB[7]])
                    for hl in range(2):
                        k.op("dve", lambda e, tb=tb, hl=hl: e.tensor_scalar(
                            out=ktok[:, tb, hl * 256:(hl + 1) * 256], in0=pst[:, hl * 256:(hl + 1) * 256],
                            scalar1=cf[:, F_GC + hl:F_GC + hl + 1], scalar2=None, op0=ALU.mult),
                            R=[PSB[7], B_cf], W=[B_ktok])
                for tb in range(4):
                    for hl in range(2):
                        bank = 2 + pp % 2
                        pp += 1
                        for kc in range(8):
                            k.op("pe", lambda e, kc=kc, tb=tb, hl=hl, bank=bank: e.matmul(
                                ps[bank][:, :], lhsT=hb[:, kc, tb * 128:(tb + 1) * 128],
                                rhs=Win[:, kc, 1024 + hl * 512:1024 + (hl + 1) * 512],
                                start=(kc == 0), stop=(kc == 7)), R=[B_Win, B_hb], W=[PSB[bank]])
                        k.op("dve", lambda e, tb=tb, hl=hl, bank=bank: e.tensor_scalar(
                            out=w[:, tb, hl, :], in0=ps[bank][:, :], scalar1=cf[:, F_SCW + hl:F_SCW + hl + 1],
                            scalar2=None, op0=ALU.mult), R=[PSB[bank], B_cf], W=[B_w])
                for tb in range(4):
                    for hl in range(2):
                        bank = 2 + pp % 2
                        pp += 1
                        for kc in range(8):
                            k.op("pe", lambda e, kc=kc, tb=tb, hl=hl, bank=bank: e.matmul(
                                ps[bank][:, :], lhsT=hb[:, kc, tb * 128:(tb + 1) * 128],
                                rhs=Win[:, kc, 2048 + hl * 512:2048 + (hl + 1) * 512],
                                start=(kc == 0), stop=(kc == 7)), R=[B_Win, B_hb], W=[PSB[bank]])
                        ti = pp % 2
                        k.op("act", lambda e, bank=bank, ti=ti: e.activation(out=tA[ti][:, :], in_=ps[bank][:, :], func=AF.Exp, scale=-1.0),
                             R=[PSB[bank]], W=[B_tA[ti]])
                        k.op("act", lambda e, ti=ti: e.activation(out=tA[ti][:, :], in_=tA[ti][:, :], func=AF.Ln, bias=1.0),
                             R=[B_tA[ti]], W=[B_tA[ti]])
                        k.op("act", lambda e, ti=ti: e.activation(out=tA[ti][:, :], in_=tA[ti][:, :], func=AF.Exp, scale=-1.0),
                             R=[B_tA[ti]], W=[B_tA[ti]])
                        k.op("dve", lambda e, tb=tb, hl=hl, bank=bank, ti=ti: e.tensor_tensor(
                            out=sg[:, tb, hl, :], in0=ps[bank][:, :], in1=tA[ti][:, :], op=ALU.mult),
                            R=[PSB[bank], B_tA[ti]], W=[B_sg])
                for tb in range(4):
                    tsl = slice(tb * 128, (tb + 1) * 128)
                    for hl in range(2):
                        psS, B_psS = (ps[4], PSB[4]) if hl == 0 else (ps[2], PSB[2])
                        psP, B_psP = (ps[5], PSB[5]) if hl == 0 else (ps[3], PSB[3])
                        psU, B_psU = (ps[6], ps[1]), (PSB[6], PSB[1])
                        for hf in range(2):
                            k.op("pe", lambda e, hl=hl, hf=hf: e.matmul(
                                psS[:, 0:128], lhsT=kTt[:, 2 * hl + hf, tsl], rhs=qT[:, 2 * hl + hf, tsl],
                                start=(hf == 0), stop=(hf == 1)), R=[B_kT, B_qT], W=[B_psS])
                        k.op("dve", lambda e: e.tensor_tensor(out=sT[hl][:, :], in0=psS[:, 0:128], in1=MLE, op=ALU.mult),
                             R=[B_psS, B_cb], W=[B_sT[hl]])
                        for hf in range(2):
                            k.op("pe", lambda e, hl=hl, hf=hf: e.matmul(
                                psP[:, :], lhsT=qT[:, 2 * hl + hf, tsl], rhs=stb[:, hl, hf, :],
                                start=(hf == 0), stop=False), R=[B_qT, B_stb[hl][hf]], W=[B_psP])
                        k.op("pe", lambda e, hl=hl, tb=tb: e.matmul(
                            psP[:, :], lhsT=sT[hl][:, :], rhs=w[:, tb, hl, :], start=False, stop=True),
                            R=[B_sT[hl], B_w], W=[B_psP])
                        for hf in range(2):
                            k.op("pe", lambda e, hl=hl, hf=hf, tb=tb: e.matmul(
                                psU[hf][:, :], lhsT=ktok[:, tb, hl * 256 + hf * 128:hl * 256 + (hf + 1) * 128],
                                rhs=w[:, tb, hl, :], start=True, stop=True), R=[B_ktok, B_w], W=[B_psU[hf]])
                            k.op("dve", lambda e, hl=hl, hf=hf: e.scalar_tensor_tensor(
                                out=stf[:, hl, hf, :], in0=stf[:, hl, hf, :], scalar=cf[:, F_GC + hl:F_GC + hl + 1],
                                in1=psU[hf][:, :], op0=ALU.mult, op1=ALU.add),
                                R=[B_psU[hf], B_stf[hl][hf], B_cf], W=[B_stf[hl][hf]])
                            k.op("act", lambda e, hl=hl, hf=hf: e.activation(out=stb[:, hl, hf, :], in_=stf[:, hl, hf, :], func=AF.Copy),
                                 R=[B_stf[hl][hf]], W=[B_stb[hl][hf]])
                        k.op("dve", lambda e: e.bn_stats(out=st6[hl][:, :], in_=psP[:, :]), R=[B_psP], W=[B_st6[hl]])
                        k.op("dve", lambda e: e.bn_aggr(out=mv[hl][:, :], in_=st6[hl][:, :]), R=[B_st6[hl]], W=[B_mv[hl]])
                        k.op("dve", lambda e, hl=hl: e.tensor_tensor(
                            out=rs[hl][:, 0:1], in0=mv[hl][:, 1:2], in1=cf[:, F_EPS + hl:F_EPS + hl + 1], op=ALU.add),
                            R=[B_mv[hl], B_cf], W=[B_rs[hl]])
                        k.op("act", lambda e: e.activation(out=rs[hl][:, 1:2], in_=rs[hl][:, 0:1], func=AF.Ln), R=[B_rs[hl]], W=[B_rs[hl]])
                        k.op("act", lambda e: e.activation(out=rs[hl][:, 0:1], in_=rs[hl][:, 1:2], func=AF.Exp, scale=-0.5), R=[B_rs[hl]], W=[B_rs[hl]])
                        k.op("dve", lambda e: e.tensor_scalar(
                            out=on[hl][:, :], in0=psP[:, :], scalar1=mv[hl][:, 0:1], scalar2=rs[hl][:, 0:1],
                            op0=ALU.subtract, op1=ALU.mult), R=[B_psP, B_mv[hl], B_rs[hl]], W=[B_on[hl]])
                        k.op("pool", lambda e, hl=hl, tb=tb: e.tensor_tensor(
                            out=ytk[:, hl * 512:(hl + 1) * 512], in0=on[hl][:, :], in1=sg[:, tb, hl, :], op=ALU.mult),
                            R=[B_on[hl], B_sg], W=[B_ytk])
                    for fc in range(8):
                        k.op("pe", lambda e, fc=fc: e.transpose(
                            out=pst[:, fc * 128:(fc + 1) * 128], in_=ytk[:, fc * 128:(fc + 1) * 128], identity=IDB),
                            R=[B_ytk, B_cb], W=[PSB[7]])
                    k.op("act", lambda e, tb=tb: e.activation(
                        out=yT[:, :, tb * 128:(tb + 1) * 128],
                        in_=pst[:, :].rearrange("p (c t) -> p c t", c=8), func=AF.Copy),
                        R=[PSB[7]], W=[B_yT])
                for oc in range(8):
                    bank = pp % 2
                    pp += 1
                    for fc in range(8):
                        k.op("pe", lambda e, oc=oc, fc=fc, bank=bank: e.matmul(
                            ps[bank][:, :], lhsT=Wo[:, fc, oc * 128:(oc + 1) * 128], rhs=yT[:, fc, :],
                            start=(fc == 0), stop=(fc == 7)), R=[B_Wo, B_yT], W=[PSB[bank]])
                    k.op("act", lambda e, oc=oc, bank=bank: e.activation(out=stage[:, oc, :], in_=ps[bank][:, :], func=AF.Copy),
                         R=[PSB[bank]], W=[B_stage])
                store_partial(T, stage, B_stage)
                reduce_scatter_tile(T)
            k.barrier()

    def phase_mlp(layer):
        last = layer == DEPTH - 1
        with contextlib.ExitStack() as es:
            def sb(name, shape, dt):
                return es.enter_context(nc.sbuf_tensor(un("ml_" + name), shape, dt))
            h = sb("h", [128, 8, TT], F32)
            dl = sb("d", [128, 8, TT], F32)
            hn = sb("hn", [128, 8, TT], BF16)
            sq = sb("sq", [128, 8, TT], BF16)
            tmp = sb("tmp", [128, TT], F32)
            rstd = sb("rstd", [128, TT], F32)
            u = sb("u", [128, 32, TT], BF16)
            rl = [sb(f"rl{i}", [128, TT], F32) for i in range(2)]
            wup = [sb(f"wup{i}", [128, 8, 1024], BF16) for i in range(2)]
            wdn = [sb(f"wdn{i}", [128, 32, 256], BF16) for i in range(2)]
            ps = [es.enter_context(nc.psum_tensor(un(f"ml_ps{i}"), [128, TT], F32)) for i in range(7)]
            B_h, B_d, B_hn, B_sq, B_tmp, B_rstd, B_u = (Buf(n) for n in ("mh", "md", "mhn", "msq", "mtmp", "mrstd", "mu"))
            B_rl = [Buf("rl0"), Buf("rl1")]
            B_wup = [Buf("wup0"), Buf("wup1")]
            B_wdn = [Buf("wdn0"), Buf("wdn1")]
            wi = 0
            wj = 0
            pp = 0
            for j in range(NOWN):
                k.dma("sp", h[:, :, :], hres_d[j], R=[B_hres[j]], W=[B_h])
                for half in range(2):
                    m = 2 * j + half
                    k.dma("sp", dl[:, :, half * 256:(half + 1) * 256],
                          rb_d[m].ap().rearrange("(c p) t -> p c t", p=128), R=[B_rb[m]], W=[B_d], nowaw=True)
                k.op("pool", lambda e: e.tensor_tensor(out=h[:, :, :], in0=h[:, :, :], in1=dl[:, :, :], op=ALU.add),
                     R=[B_h, B_d], W=[B_h])
                rmsnorm(h, B_h, 4 + layer, hn, B_hn, sq, B_sq, tmp, B_tmp, rstd, B_rstd, ps[6], PSB[6])
                for q in range(4):
                    wb, B_wb = wup[wi % 2], B_wup[wi % 2]
                    wi += 1
                    load_w(wb, wup_d[layer][:, q * 1024:(q + 1) * 1024], 8, B_wb)
                    for hcl in range(8):
                        hc = q * 8 + hcl
                        bank = pp % 2
                        pp += 1
                        for kc in range(8):
                            k.op("pe", lambda e, kc=kc, hcl=hcl, bank=bank, wb=wb: e.matmul(
                                ps[bank][:, :], lhsT=wb[:, kc, hcl * 128:(hcl + 1) * 128], rhs=hn[:, kc, :],
                                start=(kc == 0), stop=(kc == 7)), R=[B_wb, B_hn], W=[PSB[bank]])
                        k.op("act", lambda e, bank=bank: e.activation(out=rl[bank][:, :], in_=ps[bank][:, :], func=AF.Relu),
                             R=[PSB[bank]], W=[B_rl[bank]])
                        k.op("dve", lambda e, bank=bank, hc=hc: e.tensor_tensor(
                            out=u[:, hc, :], in0=rl[bank][:, :], in1=rl[bank][:, :], op=ALU.mult),
                            R=[B_rl[bank]], W=[B_u])
                for o in range(4):
                    wb, B_wb = wdn[wj % 2], B_wdn[wj % 2]
                    wj += 1
                    load_w(wb, wdn_d[layer][:, o * 256:(o + 1) * 256], 32, B_wb)
                    for ol in range(2):
                        oc = o * 2 + ol
                        bank = 2 + pp % 2
                        pp += 1
                        for hc in range(32):
                            k.op("pe", lambda e, hc=hc, ol=ol, bank=bank, wb=wb: e.matmul(
                                ps[bank][:, :], lhsT=wb[:, hc, ol * 128:(ol + 1) * 128], rhs=u[:, hc, :],
                                start=(hc == 0), stop=(hc == 31)), R=[B_wb, B_u], W=[PSB[bank]])
                        k.op("dve", lambda e, oc=oc, bank=bank: e.tensor_tensor(
                            out=h[:, oc, :], in0=h[:, oc, :], in1=ps[bank][:, :], op=ALU.add),
                            R=[PSB[bank], B_h], W=[B_h])
                if not last:
                    k.dma("sp", hres_d[j], h[:, :, :], R=[B_h], W=[B_hres[j]])
                    rmsnorm(h, B_h, layer + 1, hn, B_hn, sq, B_sq, tmp, B_tmp, rstd, B_rstd, ps[6], PSB[6])
                    emit_hn(j, hn, B_hn)
                else:
                    rmsnorm(h, B_h, 8, dl, B_d, sq, B_sq, tmp, B_tmp, rstd, B_rstd, ps[6], PSB[6])
                    for tb in range(4):
                        for half in range(2):
                            bank = 4 + half
                            for cl in range(4):
                                c = half * 4 + cl
                                k.op("pe", lambda e, tb=tb, c=c, cl=cl, bank=bank: e.transpose(
                                    out=ps[bank][:, cl * 128:(cl + 1) * 128], in_=dl[:, c, tb * 128:(tb + 1) * 128],
                                    identity=IDF), R=[B_d, B_cf], W=[PSB[bank]])
                            k.op("act", lambda e, tb=tb, half=half, bank=bank: e.activation(
                                out=h[:, tb * 2 + half, :], in_=ps[bank][:, :], func=AF.Copy),
                                R=[PSB[bank]], W=[B_h])
                    k.dma("sp", y_d[j * TT:(j + 1) * TT, :].rearrange("(tb p) f -> p tb f", p=128),
                          h[:, :, :].rearrange("p (tb x) f -> p tb (x f)", x=2), R=[B_h], W=[B_y])
            k.barrier()

    B_y = Buf("yout")
    phases = [("p0", phase0)]
    for layer in range(DEPTH):
        if layer % 2 == 0:
            phases.append((f"mix{layer}", lambda layer=layer: phase_sb(layer // 2)))
        else:
            phases.append((f"mix{layer}", lambda layer=layer: phase_ret(layer // 2)))
        phases.append((f"mlp{layer}", lambda layer=layer: phase_mlp(layer)))
    for name, fn in phases:
        fn()
        if stop_after == name:
            break
    if debug:
        dbg_h = nc.dram_tensor("dbg_h", [NOWN, 128, 8, TT], F32, kind="ExternalOutput")
        dbg_yb = nc.dram_tensor("dbg_yb", [NOWN, 2 * D, TT], BF16, kind="ExternalOutput")
        dbg_rb = nc.dram_tensor("dbg_rb", [8, 8 * 128, 256], F32, kind="ExternalOutput")
        dbg_pb = nc.dram_tensor("dbg_pb", [8, 16 * 128, 256], F32, kind="ExternalOutput")
        B_dbg = Buf("dbg")
        k.barrier(engines=("sp",))
        for j in range(NOWN):
            k.dma("sp", dbg_h[j], hres_d[j], W=[B_dbg], nowaw=True)
            k.dma("sp", dbg_yb[j], yb_d[j].ap(), W=[B_dbg], nowaw=True)
        for m in range(8):
            k.dma("sp", dbg_rb[m], rb_d[m].ap(), W=[B_dbg], nowaw=True)
            k.dma("sp", dbg_pb[m], pb_d[m].ap(), W=[B_dbg], nowaw=True)
    k.barrier(engines=("sp",))
    return nc


def _consts(p):
    j = np.arange(128)
    cbm = np.zeros((128, NCB), np.float32)
    cbm[:, C_TRI:C_TRI + 128] = (j[:, None] >= j[None, :])
    cbm[:, C_ONES:C_ONES + 128] = 1.0
    cbm[:, C_MLE:C_MLE + 128] = (j[:, None] <= j[None, :])
    cbm[:, C_ID:C_ID + 128] = np.eye(128)
    t = np.arange(512)
    for d in range(4):
        cbm[:, C_MASK + d * 512:C_MASK + (d + 1) * 512] = (j[:, None] + 128 * d < t[None, :])
    cbm[:, C_UTRI:C_UTRI + 128] = (j[:, None] < j[None, :])
    return cbm


def _cf(p, g_mix, g_mlp, g_final):
    cfm = np.zeros((128, NCF), np.float32)
    cfm[:, F_ID:F_ID + 128] = np.eye(128)
    gains = np.concatenate([g_mix, g_mlp, g_final[None, :]], axis=0)
    cfm[:, F_G:F_G + 72] = gains.reshape(9, 8, 128).transpose(2, 0, 1).reshape(128, 72)
    j = np.arange(128, dtype=np.float64)
    for hl in range(2):
        hd = 2 * p + hl
        lg = np.log1p(-np.exp2(-5.0 - hd))
        cfm[:, F_SCW + hl] = np.exp(lg * (-1.0 - j)) / 16.0
        cfm[:, F_GC + hl] = np.exp(lg * 128.0)
        cfm[:, F_EPS + hl] = GN_EPS * np.exp(lg * (-2.0 * (j + 1.0)))
    return cfm


def _rope_tables():
    half = 128
    inv_freq = (1.0 / (10000.0 ** np.linspace(0.0, 1.0, half, dtype=np.float32))).astype(np.float32)
    pos = np.arange(S, dtype=np.float32)
    ang = (inv_freq[:, None] * pos[None, :]).astype(np.float32).astype(np.float64)
    return np.cos(ang).astype(np.float32), np.sin(ang).astype(np.float32)


_NC_CACHE = {}


def make_in_maps(x, w_sb_in, w_sb_out, w_ret_in, w_ret_out, g_mix, g_mlp, w_mlp_up, w_mlp_down, g_final):
    f = lambda a: np.ascontiguousarray(np.asarray(a, dtype=np.float32))
    x, w_sb_in, w_sb_out, w_ret_in, w_ret_out = f(x), f(w_sb_in), f(w_sb_out), f(w_ret_in), f(w_ret_out)
    g_mix, g_mlp, w_mlp_up, w_mlp_down, g_final = f(g_mix), f(g_mlp), f(w_mlp_up), f(w_mlp_down), f(g_final)
    cosT, sinT = _rope_tables()
    in_maps = []
    for c in range(8):
        b, p = c // 2, c % 2
        sl = slice(512 * p, 512 * p + 512)
        wsi = np.concatenate([w_sb_in[:, :, 0:1024][:, :, sl], w_sb_in[:, :, 1024:2048][:, :, sl],
                              w_sb_in[:, :, 2048:3072][:, :, sl]], axis=2)
        wso = w_sb_out[:, sl, :]
        sl2 = slice(1024 * p, 1024 * p + 1024)
        wri = np.concatenate([w_ret_in[:, :, 0:1024][:, :, sl], w_ret_in[:, :, 1024:2048][:, :, sl],
                              w_ret_in[:, :, 2048:4096][:, :, sl2], w_ret_in[:, :, 4096:6144][:, :, sl2]], axis=2)
        wro = w_ret_out[:, sl2, :]
        in_maps.append({
            "x": f(x[b, p * OWN:(p + 1) * OWN, :]),
            "w_sb_in": f(wsi), "w_sb_out": f(wso), "w_ret_in": f(wri), "w_ret_out": f(wro),
            "w_up": w_mlp_up, "w_dn": w_mlp_down,
            "cb": _consts(p), "cf": _cf(p, g_mix, g_mlp, g_final),
            "cosT": cosT, "sinT": sinT,
        })
    return in_maps


def kernel(x, w_sb_in, w_sb_out, w_ret_in, w_ret_out, g_mix, g_mlp, w_mlp_up, w_mlp_down, g_final):
    if "nc" not in _NC_CACHE:
        _NC_CACHE["nc"] = build_program()
    nc = _NC_CACHE["nc"]
    in_maps = make_in_maps(x, w_sb_in, w_sb_out, w_ret_in, w_ret_out, g_mix, g_mlp, w_mlp_up, w_mlp_down, g_final)
    res = run_bass_kernel_spmd(nc, in_maps, core_ids=list(range(8)))
    out = np.empty((NB, S, D), np.float32)
    for c in range(8):
        b, p = c // 2, c % 2
        out[b, p * OWN:(p + 1) * OWN, :] = np.asarray(res.results[c]["y"], dtype=np.float32)
    return out
```

```python
import contextlib
import numpy as np
import concourse.bass as bass
import concourse.mybir as mybir
from concourse.bass_utils import run_bass_kernel_spmd

F32 = mybir.dt.float32
BF16 = mybir.dt.bfloat16
AF = mybir.ActivationFunctionType
ALU = mybir.AluOpType

D = 1024
S = 4096
NB = 4
DEPTH = 4
OWN = 2048
TT = 512
NOWN = OWN // TT
NALL = S // TT
HID = 4096
GROUPS = [[0, 1], [2, 3], [4, 5], [6, 7]]
RMS_EPS = 1e-6
SB_TMAX = 8
SB_ATT = True
SB_RS = True
SB_LVL = 9
GN_EPS = 1e-6

C_TRI, C_ONES, C_MLE, C_ID, C_MASK = 0, 128, 256, 384, 512
C_UTRI = 512 + 4 * 512
NCB = C_UTRI + 128
F_ID, F_G, F_SCW, F_GC, F_EPS = 0, 128, 200, 202, 204
NCF = 206


class Buf:
    __slots__ = ("name", "w", "r", "dsem", "dcnt", "key", "excl")

    def __init__(self, name, excl=False):
        self.name = name
        self.excl = excl
        self.w = None
        self.r = {}
        self.dsem = None
        self.dcnt = 0


class KB:
    def __init__(self, nc):
        self.nc = nc
        self.E = {"pe": nc.tensor, "act": nc.scalar, "dve": nc.vector, "pool": nc.gpsimd, "sp": nc.sync}
        self.sem = {e: nc.alloc_semaphore("c_" + e) for e in ("pe", "act", "dve", "pool")}
        self.cnt = {e: 0 for e in self.sem}
        self.seen = {e: {} for e in self.E}
        self.latest = {}
        self.nsem = 4

    def _deps(self, R, W, nowaw=False):
        toks = []
        for b in R:
            if b.w is not None:
                toks.append(b.w)
            if b.excl:
                toks.extend(b.r.values())
        for b in W:
            if b.w is not None and not (nowaw and b.w[3] == "dma"):
                toks.append(b.w)
            toks.extend(b.r.values())
        return toks

    def _wait(self, e, toks):
        for (key, sem, val, prod) in toks:
            if prod == "pe" and e == "pe":
                continue
            if self.seen[e].get(key, 0) < val:
                self.E[e].wait_ge(sem, val)
                self.seen[e][key] = val

    def _commit(self, tok, R, W):
        self.latest[tok[0]] = tok
        for b in R:
            b.r[tok[0]] = tok
        for b in W:
            b.w = tok
            b.r = {}

    def op(self, e, fn, R=(), W=()):
        self._wait(e, self._deps(R, W))
        inst = fn(self.E[e])
        self.cnt[e] += 1
        inst.then_inc(self.sem[e], 1)
        self._commit(("c_" + e, self.sem[e], self.cnt[e], e), R, W)

    def _dsem(self, dst):
        if dst.dsem is None:
            self.nsem += 1
            dst.key = "d%d_%s" % (self.nsem, dst.name)
            dst.dsem = self.nc.alloc_semaphore(dst.key)
        return dst.dsem

    def dma(self, q, out, in_, R=(), W=(), nowaw=False):
        dst = W[0]
        self._wait(q, self._deps(R, W, nowaw))
        sem = self._dsem(dst)
        inst = self.E[q].dma_start(out=out, in_=in_)
        dst.dcnt += 16
        inst.then_inc(sem, 16)
        self._commit((dst.key, sem, dst.dcnt, "dma"), R, W)

    def cc(self, kind, alu, in_ap, out_ap, R=(), W=()):
        dst = W[0]
        self._wait("pool", self._deps(R, W))
        sem = self._dsem(dst)
        inst = self.nc.gpsimd.collective_compute(kind, alu, replica_groups=GROUPS, ins=[in_ap], outs=[out_ap])
        dst.dcnt += 1
        inst.then_inc(sem)
        self._commit((dst.key, sem, dst.dcnt, "cc"), R, W)

    def barrier(self, engines=("pe", "act", "dve", "pool", "sp")):
        toks = [t for t in self.latest.values() if t[3] != "cc" or len(engines) == 1]
        for e in engines:
            self._wait(e, toks)


def build_program(stop_after=None, debug=False):
    nc = bass.Bass("TRN2", target_bir_lowering=False)
    k = KB(nc)

    _uid = [0]

    def un(name):
        _uid[0] += 1
        return "%s_%d" % (name, _uid[0])

    def dram_in(name, shape, dt=F32):
        return nc.dram_tensor(name, shape, dt, kind="ExternalInput")

    x_d = dram_in("x", [OWN, D])
    wsi_d = dram_in("w_sb_in", [2, D, 1536])
    wso_d = dram_in("w_sb_out", [2, 512, D])
    wri_d = dram_in("w_ret_in", [2, D, 3072])
    wro_d = dram_in("w_ret_out", [2, 1024, D])
    wup_d = dram_in("w_up", [DEPTH, D, HID])
    wdn_d = dram_in("w_dn", [DEPTH, HID, D])
    cb_d = dram_in("cb", [128, NCB])
    cf_d = dram_in("cf", [128, NCF])
    cos_d = dram_in("cosT", [128, S])
    sin_d = dram_in("sinT", [128, S])
    y_d = nc.dram_tensor("y", [OWN, D], F32, kind="ExternalOutput")

    hres_d = nc.dram_tensor("hres", [NOWN, 128, 8, TT], F32)
    xb_d = [nc.dram_tensor(f"xb{j}", [D, TT], BF16) for j in range(NOWN)]
    yb_d = [nc.dram_tensor(f"yb{j}", [2 * D, TT], BF16) for j in range(NOWN)]
    pb_d = [nc.dram_tensor(f"pb{m}", [2 * 8 * 128, 256], F32) for m in range(8)]
    rb_d = [nc.dram_tensor(f"rb{m}", [8 * 128, 256], F32) for m in range(8)]
    B_hres = [Buf(f"hres{j}") for j in range(NOWN)]
    B_xb = [Buf(f"xb{j}") for j in range(NOWN)]
    B_yb = [Buf(f"yb{j}") for j in range(NOWN)]
    B_pb = [Buf(f"pb{m}") for m in range(8)]
    B_rb = [Buf(f"rb{m}") for m in range(8)]

    cb = nc.alloc_sbuf_tensor("cb_s", [128, NCB], BF16)
    cf = nc.alloc_sbuf_tensor("cf_s", [128, NCF], F32)
    B_cb, B_cf = Buf("cb"), Buf("cf")
    k.dma("pool", cb[:, :], cb_d[:, :], W=[B_cb])
    k.dma("sp", cf[:, :], cf_d[:, :], W=[B_cf])
    TRI = cb[:, C_TRI:C_TRI + 128]
    ONES = cb[:, C_ONES:C_ONES + 128]
    UTRI = cb[:, C_UTRI:C_UTRI + 128]
    MLE = cb[:, C_MLE:C_MLE + 128]
    IDB = cb[:, C_ID:C_ID + 128]
    IDF = cf[:, F_ID:F_ID + 128]

    def load_w(dst, src2d, nk, B_dst):
        for kc in range(nk):
            k.dma("pool", dst[:, kc, :], src2d[kc * 128:(kc + 1) * 128, :], W=[B_dst], nowaw=True)

    def maskd(d):
        return cb[:, C_MASK + d * 512:C_MASK + (d + 1) * 512]

    PSB = [Buf(f"ps{i}", excl=True) for i in range(8)]

    def rmsnorm(h, B_h, gidx, out, B_out, sq, B_sq, tmp, B_tmp, rstd, B_rstd, ps, B_ps):
        k.op("act", lambda e: e.activation(out=sq[:, :, :], in_=h[:, :, :], func=AF.Square),
             R=[B_h], W=[B_sq])
        for c in range(8):
            k.op("pe", lambda e, c=c: e.matmul(ps[:, :], lhsT=ONES, rhs=sq[:, c, :], start=(c == 0), stop=(c == 7)),
                 R=[B_sq, B_cb], W=[B_ps])
        k.op("act", lambda e: e.activation(out=tmp[:, :], in_=ps[:, :], func=AF.Ln, scale=1.0 / D, bias=RMS_EPS),
             R=[B_ps], W=[B_tmp])
        k.op("act", lambda e: e.activation(out=rstd[:, :], in_=tmp[:, :], func=AF.Exp, scale=-0.5),
             R=[B_tmp], W=[B_rstd])
        for c in range(8):
            k.op("dve", lambda e, c=c: e.scalar_tensor_tensor(
                out=out[:, c, :], in0=h[:, c, :], scalar=cf[:, F_G + gidx * 8 + c:F_G + gidx * 8 + c + 1],
                in1=rstd[:, :], op0=ALU.mult, op1=ALU.mult), R=[B_h, B_rstd, B_cf], W=[B_out])

    def emit_hn(j, hn, B_hn):
        k.dma("sp", xb_d[j].ap().rearrange("(c p) t -> p c t", p=128), hn[:, :, :], R=[B_hn], W=[B_xb[j]])
        k.cc("AllGather", ALU.bypass, xb_d[j].ap(), yb_d[j].ap(), R=[B_xb[j]], W=[B_yb[j]])

    def load_hn(T, hn, B_hn):
        j, r = T % NOWN, T // NOWN
        k.dma("sp", hn[:, :, :], yb_d[j][r * D:(r + 1) * D, :].rearrange("(c p) t -> p c t", p=128),
              R=[B_yb[j]], W=[B_hn])

    def store_partial(T, stage, B_stage):
        j, r = T % NOWN, T // NOWN
        for half in range(2):
            m = 2 * j + half
            k.dma("sp", pb_d[m][r * 1024:(r + 1) * 1024, :].rearrange("(c p) t -> p c t", p=128),
                  stage[:, :, half * 256:(half + 1) * 256], R=[B_stage], W=[B_pb[m]], nowaw=True)

    def reduce_scatter_tile(T):
        if T >= NOWN:
            for m in (2 * (T - NOWN), 2 * (T - NOWN) + 1):
                k.cc("ReduceScatter", ALU.add, pb_d[m].ap(), rb_d[m].ap(), R=[B_pb[m]], W=[B_rb[m]])

    def phase0():
        with contextlib.ExitStack() as es:
            X = es.enter_context(nc.sbuf_tensor("p0_X", [128, 4, D], F32))
            h = es.enter_context(nc.sbuf_tensor("p0_h", [128, 8, TT], F32))
            hn = es.enter_context(nc.sbuf_tensor("p0_hn", [128, 8, TT], BF16))
            sq = es.enter_context(nc.sbuf_tensor("p0_sq", [128, 8, TT], BF16))
            tmp = es.enter_context(nc.sbuf_tensor("p0_tmp", [128, TT], F32))
            rstd = es.enter_context(nc.sbuf_tensor("p0_rstd", [128, TT], F32))
            ps = [es.enter_context(nc.psum_tensor(f"p0_ps{i}", [128, TT], F32)) for i in range(3)]
            B_X, B_h, B_hn, B_sq, B_tmp, B_rstd = (Buf(n) for n in ("p0X", "p0h", "p0hn", "p0sq", "p0tmp", "p0rstd"))
            for j in range(NOWN):
                k.dma("sp", X[:, :, :], x_d[j * TT:(j + 1) * TT, :].rearrange("(tb p) f -> p tb f", p=128), W=[B_X])
                for c in range(8):
                    pb_ = ps[c % 2]
                    for tb in range(4):
                        k.op("pe", lambda e, c=c, tb=tb, pb_=pb_: e.transpose(
                            out=pb_[:, tb * 128:(tb + 1) * 128], in_=X[:, tb, c * 128:(c + 1) * 128], identity=IDF),
                            R=[B_X, B_cf], W=[PSB[c % 2]])
                    k.op("act", lambda e, c=c, pb_=pb_: e.activation(out=h[:, c, :], in_=pb_[:, :], func=AF.Copy),
                         R=[PSB[c % 2]], W=[B_h])
                k.dma("sp", hres_d[j], h[:, :, :], R=[B_h], W=[B_hres[j]])
                rmsnorm(h, B_h, 0, hn, B_hn, sq, B_sq, tmp, B_tmp, rstd, B_rstd, ps[2], PSB[2])
                emit_hn(j, hn, B_hn)
            k.barrier()

    def phase_sb(li):
        with contextlib.ExitStack() as es:
            def sb(name, shape, dt):
                return es.enter_context(nc.sbuf_tensor(un("sb_" + name), shape, dt))
            Win = sb("Win", [128, 8, 1536], BF16)
            Wo = sb("Wo", [128, 4, D], BF16)
            kT = sb("kT", [128, 4, S], BF16)
            vA = sb("vA", [128, 32, 512], BF16)
            hn = [sb(f"hn{i}", [128, 8, TT], BF16) for i in range(2)]
            qT = sb("qT", [128, 4, TT], BF16)
            Ee = [sb(f"E{i}", [128, 2 * TT], F32) for i in range(3)]
            sp = [sb(f"sp{i}", [128, 2 * TT], BF16) for i in range(2)]
            Gg = sb("G", [128, 2 * TT], F32)
            accf = sb("accf", [128, 2 * TT], F32)
            accb = [sb(f"accb{i}", [128, 2 * TT], BF16) for i in range(2)]
            At = [sb(f"At{i}", [128, 2 * TT], BF16) for i in range(2)]
            oT = sb("oT", [128, 4, TT], BF16)
            stage = sb("stage", [128, 8, TT], F32)
            ps = [es.enter_context(nc.psum_tensor(un(f"sb_ps{i}"), [128, TT], F32)) for i in range(2)]
            Zab = es.enter_context(nc.psum_tensor(un("sb_Z"), [128, 2 * TT], F32))
            Cab = es.enter_context(nc.psum_tensor(un("sb_C"), [128, 2 * TT], F32))
            Oab = es.enter_context(nc.psum_tensor(un("sb_O"), [128, 2 * TT], F32))
            B_Win, B_Wo = Buf("sbWin"), Buf("sbWo")
            B_kT = [Buf(f"kT{i}") for i in range(NALL)]
            B_v = [Buf(f"v{i}") for i in range(NALL)]
            B_hn = [Buf("sbhn0"), Buf("sbhn1")]
            B_qT, B_nqT, B_oT, B_stage = Buf("qT"), Buf("nqT"), Buf("oT"), Buf("sbstage")
            B_E = [Buf("E0"), Buf("E1"), Buf("E2")]
            B_sp = [Buf("sp0"), Buf("sp1")]
            B_G = Buf("G")
            B_accf = Buf("accf")
            B_accb = [Buf("accb0"), Buf("accb1")]
            B_At = [Buf("At0"), Buf("At1")]

            load_w(Win, wsi_d[li], 8, B_Win)
            load_w(Wo, wso_d[li], 4, B_Wo)
            load_hn(0, hn[0], B_hn[0])
            pp = 0
            for T in range(SB_TMAX):
                hb, B_hb = hn[T % 2], B_hn[T % 2]
                if T + 1 < NALL:
                    load_hn(T + 1, hn[(T + 1) % 2], B_hn[(T + 1) % 2])
                for pr in range(4):
                    for which in range(2):
                        bank = pp % 2
                        pp += 1
                        col0 = which * 512 + pr * 128
                        for kc in range(8):
                            k.op("pe", lambda e, kc=kc, col0=col0, bank=bank: e.matmul(
                                ps[bank][:, :], lhsT=Win[:, kc, col0:col0 + 128], rhs=hb[:, kc, :],
                                start=(kc == 0), stop=(kc == 7)), R=[B_Win, B_hb], W=[PSB[bank]])
                        if which == 0:
                            k.op("dve", lambda e, pr=pr, bank=bank: e.tensor_copy(out=qT[:, pr, :], in_=ps[bank][:, :]),
                                 R=[PSB[bank]], W=[B_qT])
                        else:
                            k.op("act", lambda e, pr=pr, bank=bank: e.activation(
                                out=kT[:, pr, T * TT:(T + 1) * TT], in_=ps[bank][:, :], func=AF.Copy),
                                R=[PSB[bank]], W=[B_kT[T]])
                for tb in range(4 if SB_LVL >= 2 else 0):
                    bank = pp % 2
                    pp += 1
                    for kc in range(8):
                        k.op("pe", lambda e, kc=kc, tb=tb, bank=bank: e.matmul(
                            ps[bank][:, :], lhsT=hb[:, kc, tb * 128:(tb + 1) * 128], rhs=Win[:, kc, 1024:1536],
                            start=(kc == 0), stop=(kc == 7)), R=[B_Win, B_hb], W=[PSB[bank]])
                    k.op("dve", lambda e, tb=tb, bank=bank: e.tensor_copy(out=vA[:, T * 4 + tb, :], in_=ps[bank][:, :]),
                         R=[PSB[bank]], W=[B_v[T]])
                nst = 4 * T + 4
                BZ = (PSB[2], PSB[3]); BC = (PSB[4], PSB[5]); BO = (PSB[6], PSB[7])
                for pr in range(4 if SB_ATT else 0):
                    offs = (0, 64)

                    def hs(s_):
                        return slice(s_ * TT, (s_ + 1) * TT)

                    def zmm(i):
                        kb = 4 * T + 3 - i
                        for s_ in range(2):
                            o_ = offs[s_]
                            k.op("pe", lambda e: e.matmul(
                                Zab[:, hs(s_)], lhsT=kT[o_:o_ + 64, pr, kb * 128:(kb + 1) * 128],
                                rhs=qT[o_:o_ + 64, pr, :], start=True, stop=True),
                                R=[B_kT[kb // 4], B_qT], W=[BZ[s_]])

                    def act_E(i):
                        k.op("act", lambda e: e.activation(out=Ee[i % 3][:, :], in_=Zab[:, :], func=AF.Exp, scale=0.125),
                             R=[BZ[0], BZ[1]], W=[B_E[i % 3]])

                    def avmm(i):
                        kb = 4 * T + 3 - i
                        par = i % 2
                        for s_ in range(2):
                            k.op("pe", lambda e: e.matmul(
                                Oab[:, hs(s_)], lhsT=vA[:, kb, pr * 128:(pr + 1) * 128], rhs=At[par][:, hs(s_)],
                                start=(i == 0), stop=(i == nst - 1)), R=[B_v[kb // 4], B_At[par]], W=[BO[s_]])

                    zmm(0)
                    act_E(0)
                    for i in range(nst):
                        kb = 4 * T + 3 - i
                        dz = kb - 4 * T
                        par = i % 2
                        if i + 1 < nst:
                            zmm(i + 1)
                        k.op("act", lambda e: e.activation(out=sp[par][:, :], in_=Ee[i % 3][:, :], func=AF.Ln, bias=1.0),
                             R=[B_E[i % 3]], W=[B_sp[par]])
                        if dz >= 0:
                            for s_ in range(2):
                                k.op("pool", lambda e: e.tensor_tensor(
                                    out=sp[par][:, hs(s_)], in0=sp[par][:, hs(s_)], in1=maskd(dz), op=ALU.mult),
                                    R=[B_sp[par], B_cb], W=[B_sp[par]])
                        if i + 1 < nst:
                            act_E(i + 1)
                        for s_ in range(2):
                            k.op("pe", lambda e: e.matmul(Cab[:, hs(s_)], lhsT=TRI, rhs=sp[par][:, hs(s_)],
                                                          start=True, stop=(i == 0)),
                                 R=[B_sp[par], B_cb], W=[BC[s_]])
                            if i > 0:
                                k.op("pe", lambda e: e.matmul(Cab[:, hs(s_)], lhsT=ONES, rhs=accb[par][:, hs(s_)],
                                                              start=False, stop=True),
                                     R=[B_accb[par], B_cb], W=[BC[s_]])
                        k.op("act", lambda e: e.activation(out=Gg[:, :], in_=Cab[:, :], func=AF.Exp, scale=-1.0),
                             R=[BC[0], BC[1]], W=[B_G])
                        if i > 0:
                            avmm(i - 1)
                        if i + 1 < nst:
                            if i == 0:
                                k.op("dve", lambda e: e.tensor_copy(out=accf[:, :], in_=sp[par][:, :]),
                                     R=[B_sp[par]], W=[B_accf])
                            else:
                                k.op("dve", lambda e: e.tensor_tensor(out=accf[:, :], in0=accf[:, :], in1=sp[par][:, :], op=ALU.add),
                                     R=[B_sp[par], B_accf], W=[B_accf])
                            k.op("dve", lambda e: e.tensor_copy(out=accb[1 - par][:, :], in_=accf[:, :]),
                                 R=[B_accf], W=[B_accb[1 - par]])
                        k.op("dve", lambda e: e.tensor_tensor(out=At[par][:, :], in0=Ee[i % 3][:, :], in1=Gg[:, :], op=ALU.mult),
                             R=[B_E[i % 3], B_G], W=[B_At[par]])
                        if dz >= 0:
                            for s_ in range(2):
                                k.op("pool", lambda e: e.tensor_tensor(
                                    out=At[par][:, hs(s_)], in0=At[par][:, hs(s_)], in1=maskd(dz), op=ALU.mult),
                                    R=[B_At[par], B_cb], W=[B_At[par]])
                    avmm(nst - 1)
                    for s_ in range(2):
                        o_ = offs[s_]
                        k.op("dve", lambda e: e.tensor_copy(out=oT[o_:o_ + 64, pr, :], in_=Oab[o_:o_ + 64, hs(s_)]),
                             R=[BO[s_]], W=[B_oT])
                for oc in range(8 if SB_LVL >= 3 else 0):
                    bank = pp % 2
                    pp += 1
                    for pc in range(4):
                        k.op("pe", lambda e, oc=oc, pc=pc, bank=bank: e.matmul(
                            ps[bank][:, :], lhsT=Wo[:, pc, oc * 128:(oc + 1) * 128], rhs=oT[:, pc, :],
                            start=(pc == 0), stop=(pc == 3)), R=[B_Wo, B_oT], W=[PSB[bank]])
                    k.op("act", lambda e, oc=oc, bank=bank: e.activation(out=stage[:, oc, :], in_=ps[bank][:, :], func=AF.Copy),
                         R=[PSB[bank]], W=[B_stage])
                if SB_LVL >= 4:
                    store_partial(T, stage, B_stage)
                    if SB_RS:
                        reduce_scatter_tile(T)
            k.barrier()

    def phase_ret(li):
        with contextlib.ExitStack() as es:
            def sb(name, shape, dt):
                return es.enter_context(nc.sbuf_tensor(un("rt_" + name), shape, dt))
            Win = sb("Win", [128, 8, 3072], BF16)
            Wo = sb("Wo", [128, 8, D], BF16)
            hn = [sb(f"hn{i}", [128, 8, TT], BF16) for i in range(2)]
            csb = [sb(f"cos{i}", [128, TT], F32) for i in range(2)]
            snb = [sb(f"sin{i}", [128, TT], F32) for i in range(2)]
            qT = sb("qT", [128, 4, TT], BF16)
            kTt = sb("kT", [128, 4, TT], BF16)
            ktok = sb("ktok", [128, 4, 512], BF16)
            w = sb("w", [128, 4, 2, 512], BF16)
            sg = sb("sg", [128, 4, 2, 512], BF16)
            tA = [sb(f"tA{i}", [128, TT], F32) for i in range(4)]
            sT = [sb(f"sT{i}", [128, 128], BF16) for i in range(2)]
            stf = sb("stf", [128, 2, 2, 512], F32)
            stb = sb("stb", [128, 2, 2, 512], BF16)
            on = [sb(f"on{i}", [128, 512], F32) for i in range(2)]
            ytk = sb("ytk", [128, 1024], BF16)
            yT = sb("yT", [128, 8, TT], BF16)
            stage = sb("stage", [128, 8, TT], F32)
            st6 = [sb(f"st6{i}", [128, 6], F32) for i in range(2)]
            mv = [sb(f"mv{i}", [128, 2], F32) for i in range(2)]
            rs = [sb(f"rs{i}", [128, 2], F32) for i in range(2)]
            ps = [es.enter_context(nc.psum_tensor(un(f"rt_ps{i}"), [128, TT], F32)) for i in range(7)]
            pst = es.enter_context(nc.psum_tensor(un("rt_pst"), [128, 1024], BF16))
            B_Win, B_Wo = Buf("rtWin"), Buf("rtWo")
            B_hn = [Buf("rthn0"), Buf("rthn1")]
            B_qT, B_kT, B_ktok, B_w, B_sg = (Buf(n) for n in ("rqT", "rkT", "ktok", "w", "sg"))
            B_csb = [Buf("cs0"), Buf("cs1")]
            B_snb = [Buf("sn0"), Buf("sn1")]
            B_tA = [Buf(f"tA{i}") for i in range(4)]
            B_ytk, B_yT, B_stage = (Buf(n) for n in ("ytk", "yT", "rstage"))
            B_sT, B_on, B_st6, B_mv, B_rs = ([Buf(n + "0"), Buf(n + "1")] for n in ("sT", "on", "st6", "mv", "rs"))
            B_stf = [[Buf(f"stf{a}{b}") for b in range(2)] for a in range(2)]
            B_stb = [[Buf(f"stb{a}{b}") for b in range(2)] for a in range(2)]

            load_w(Win, wri_d[li], 8, B_Win)
            load_w(Wo, wro_d[li], 8, B_Wo)
            for hl in range(2):
                for hf in range(2):
                    k.op("dve", lambda e, hl=hl, hf=hf: e.memset(stf[:, hl, hf, :], 0.0), W=[B_stf[hl][hf]])
                    k.op("dve", lambda e, hl=hl, hf=hf: e.memset(stb[:, hl, hf, :], 0.0), W=[B_stb[hl][hf]])
            load_hn(0, hn[0], B_hn[0])
            pp = 0
            for T in range(NALL):
                hb, B_hb = hn[T % 2], B_hn[T % 2]
                if T + 1 < NALL:
                    load_hn(T + 1, hn[(T + 1) % 2], B_hn[(T + 1) % 2])
                if T == 0:
                    k.dma("sp", csb[0][:, :], cos_d[:, 0:TT], W=[B_csb[0]])
                    k.dma("sp", snb[0][:, :], sin_d[:, 0:TT], W=[B_snb[0]])
                if T + 1 < NALL:
                    k.dma("sp", csb[(T + 1) % 2][:, :], cos_d[:, (T + 1) * TT:(T + 2) * TT], W=[B_csb[(T + 1) % 2]])
                    k.dma("sp", snb[(T + 1) % 2][:, :], sin_d[:, (T + 1) * TT:(T + 2) * TT], W=[B_snb[(T + 1) % 2]])
                cs, sn, B_cs, B_sn = csb[T % 2], snb[T % 2], B_csb[T % 2], B_snb[T % 2]
                for which in range(2):
                    dst, B_dst = (qT, B_qT) if which == 0 else (kTt, B_kT)
                    for hl in range(2):
                        for hf in range(2):
                            col0 = which * 512 + hl * 256 + hf * 128
                            for kc in range(8):
                                k.op("pe", lambda e, kc=kc, col0=col0, hf=hf: e.matmul(
                                    ps[hf][:, :], lhsT=Win[:, kc, col0:col0 + 128], rhs=hb[:, kc, :],
                                    start=(kc == 0), stop=(kc == 7)), R=[B_Win, B_hb], W=[PSB[hf]])
                        x1, x2 = ps[0], ps[1]
                        k.op("dve", lambda e: e.tensor_tensor(out=tA[0][:, :], in0=x1[:, :], in1=cs[:, :], op=ALU.mult),
                             R=[PSB[0], B_cs], W=[B_tA[0]])
                        k.op("dve", lambda e: e.tensor_tensor(out=tA[1][:, :], in0=x2[:, :], in1=sn[:, :], op=ALU.mult),
                             R=[PSB[1], B_sn], W=[B_tA[1]])
                        k.op("dve", lambda e: e.tensor_tensor(out=tA[2][:, :], in0=x1[:, :], in1=sn[:, :], op=ALU.mult),
                             R=[PSB[0], B_sn], W=[B_tA[2]])
                        k.op("dve", lambda e: e.tensor_tensor(out=tA[3][:, :], in0=x2[:, :], in1=cs[:, :], op=ALU.mult),
                             R=[PSB[1], B_cs], W=[B_tA[3]])
                        k.op("pool", lambda e, hl=hl, dst=dst: e.tensor_tensor(
                            out=dst[:, 2 * hl, :], in0=tA[0][:, :], in1=tA[1][:, :], op=ALU.subtract),
                            R=[B_tA[0], B_tA[1]], W=[B_dst])
                        k.op("pool", lambda e, hl=hl, dst=dst: e.tensor_tensor(
                            out=dst[:, 2 * hl + 1, :], in0=tA[2][:, :], in1=tA[3][:, :], op=ALU.add),
                            R=[B_tA[2], B_tA[3]], W=[B_dst])
                for tb in range(4):
                    for ch in range(4):
                        k.op("pe", lambda e, tb=tb, ch=ch: e.transpose(
                            out=pst[:, ch * 128:(ch + 1) * 128], in_=kTt[:, ch, tb * 128:(tb + 1) * 128], identity=IDB),
                            R=[B_kT, B_cb], W=[PSB[7]])
                    for hl in range(2):
                        k.op("dve", lambda e, tb=tb, hl=hl: e.tensor_scalar(
                            out=ktok[:, tb, hl * 256:(hl + 1) * 256], in0=pst[:, hl * 256:(hl + 1) * 256],
                            scalar1=cf[:, F_GC + hl:F_GC + hl + 1], scalar2=None, op0=ALU.mult),
                            R=[PSB[7], B_cf], W=[B_ktok])
                for tb in range(4):
                    for hl in range(2):
                        bank = 2 + pp % 2
                        pp += 1
                        for kc in range(8):
                            k.op("pe", lambda e, kc=kc, tb=tb, hl=hl, bank=bank: e.matmul(
                                ps[bank][:, :], lhsT=hb[:, kc, tb * 128:(tb + 1) * 128],
                                rhs=Win[:, kc, 1024 + hl * 512:1024 + (hl + 1) * 512],
                                start=(kc == 0), stop=(kc == 7)), R=[B_Win, B_hb], W=[PSB[bank]])
                        k.op("dve", lambda e, tb=tb, hl=hl, bank=bank: e.tensor_scalar(
                            out=w[:, tb, hl, :], in0=ps[bank][:, :], scalar1=cf[:, F_SCW + hl:F_SCW + hl + 1],
                            scalar2=None, op0=ALU.mult), R=[PSB[bank], B_cf], W=[B_w])
                for tb in range(4):
                    for hl in range(2):
                        bank = 2 + pp % 2
                        pp += 1
                        for kc in range(8):
                            k.op("pe", lambda e, kc=kc, tb=tb, hl=hl, bank=bank: e.matmul(
                                ps[bank][:, :], lhsT=hb[:, kc, tb * 128:(tb + 1) * 128],
                                rhs=Win[:, kc, 2048 + hl * 512:2048 + (hl + 1) * 512],
                                start=(kc == 0), stop=(kc == 7)), R=[B_Win, B_hb], W=[PSB[bank]])
                        ti = pp % 2
                        k.op("act", lambda e, bank=bank, ti=ti: e.activation(out=tA[ti][:, :], in_=ps[bank][:, :], func=AF.Exp, scale=-1.0),
                             R=[PSB[bank]], W=[B_tA[ti]])
                        k.op("act", lambda e, ti=ti: e.activation(out=tA[ti][:, :], in_=tA[ti][:, :], func=AF.Ln, bias=1.0),
                             R=[B_tA[ti]], W=[B_tA[ti]])
                        k.op("act", lambda e, ti=ti: e.activation(out=tA[ti][:, :], in_=tA[ti][:, :], func=AF.Exp, scale=-1.0),
                             R=[B_tA[ti]], W=[B_tA[ti]])
                        k.op("dve", lambda e, tb=tb, hl=hl, bank=bank, ti=ti: e.tensor_tensor(
                            out=sg[:, tb, hl, :], in0=ps[bank][:, :], in1=tA[ti][:, :], op=ALU.mult),
                            R=[PSB[bank], B_tA[ti]], W=[B_sg])
                for tb in range(4):
                    tsl = slice(tb * 128, (tb + 1) * 128)
                    for hl in range(2):
                        psS, B_psS = (ps[4], PSB[4]) if hl == 0 else (ps[2], PSB[2])
                        psP, B_psP = (ps[5], PSB[5]) if hl == 0 else (ps[3], PSB[3])
                        psU, B_psU = (ps[6], ps[1]), (PSB[6], PSB[1])
                        for hf in range(2):
                            k.op("pe", lambda e, hl=hl, hf=hf: e.matmul(
                                psS[:, 0:128], lhsT=kTt[:, 2 * hl + hf, tsl], rhs=qT[:, 2 * hl + hf, tsl],
                                start=(hf == 0), stop=(hf == 1)), R=[B_kT, B_qT], W=[B_psS])
                        k.op("dve", lambda e: e.tensor_tensor(out=sT[hl][:, :], in0=psS[:, 0:128], in1=MLE, op=ALU.mult),
                             R=[B_psS, B_cb], W=[B_sT[hl]])
                        for hf in range(2):
                            k.op("pe", lambda e, hl=hl, hf=hf: e.matmul(
                                psP[:, :], lhsT=qT[:, 2 * hl + hf, tsl], rhs=stb[:, hl, hf, :],
                                start=(hf == 0), stop=False), R=[B_qT, B_stb[hl][hf]], W=[B_psP])
                        k.op("pe", lambda e, hl=hl, tb=tb: e.matmul(
                            psP[:, :], lhsT=sT[hl][:, :], rhs=w[:, tb, hl, :], start=False, stop=True),
                            R=[B_sT[hl], B_w], W=[B_psP])
                        for hf in range(2):
                            k.op("pe", lambda e, hl=hl, hf=hf, tb=tb: e.matmul(
                                psU[hf][:, :], lhsT=ktok[:, tb, hl * 256 + hf * 128:hl * 256 + (hf + 1) * 128],
                                rhs=w[:, tb, hl, :], start=True, stop=True), R=[B_ktok, B_w], W=[B_psU[hf]])
                            k.op("dve", lambda e, hl=hl, hf=hf: e.scalar_tensor_tensor(
                                out=stf[:, hl, hf, :], in0=stf[:, hl, hf, :], scalar=cf[:, F_GC + hl:F_GC + hl + 1],
                                in1=psU[hf][:, :], op0=ALU.mult, op1=ALU.add),
                                R=[B_psU[hf], B_stf[hl][hf], B_cf], W=[B_stf[hl][hf]])
                            k.op("act", lambda e, hl=hl, hf=hf: e.activation(out=stb[:, hl, hf, :], in_=stf[:, hl, hf, :], func=AF.Copy),
                                 R=[B_stf[hl][hf]], W=[B_stb[hl][hf]])
                        k.op("dve", lambda e: e.bn_stats(out=st6[hl][:, :], in_=psP[:, :]), R=[B_psP], W=[B_st6[hl]])
                        k.op("dve", lambda e: e.bn_aggr(out=mv[hl][:, :], in_=st6[hl][:, :]), R=[B_st6[hl]], W=[B_mv[hl]])
                        k.op("dve", lambda e, hl=hl: e.tensor_tensor(
                            out=rs[hl][:, 0:1], in0=mv[hl][:, 1:2], in1=cf[:, F_EPS + hl:F_EPS + hl + 1], op=ALU.add),
                            R=[B_mv[hl], B_cf], W=[B_rs[hl]])
                        k.op("act", lambda e: e.activation(out=rs[hl][:, 1:2], in_=rs[hl][:, 0:1], func=AF.Ln), R=[B_rs[hl]], W=[B_rs[hl]])
                        k.op("act", lambda e: e.activation(out=rs[hl][:, 0:1], in_=rs[hl][:, 1:2], func=AF.Exp, scale=-0.5), R=[B_rs[hl]], W=[B_rs[hl]])
                        k.op("dve", lambda e: e.tensor_scalar(
                            out=on[hl][:, :], in0=psP[:, :], scalar1=mv[hl][:, 0:1], scalar2=rs[hl][:, 0:1],
                            op0=ALU.subtract, op1=ALU.mult), R=[B_psP, B_mv[hl], B_rs[hl]], W=[B_on[hl]])
                        k.op("pool", lambda e, hl=hl, tb=tb: e.tensor_tensor(
                            out=ytk[:, hl * 512:(hl + 1) * 512], in0=on[hl][:, :], in1=sg[:, tb, hl, :], op=ALU.mult),
                            R=[B_on[hl], B_sg], W=[B_ytk])
                    for fc in range(8):
                        k.op("pe", lambda e, fc=fc: e.transpose(
                            out=pst[:, fc * 128:(fc + 1) * 128], in_=ytk[:, fc * 128:(fc + 1) * 128], identity=IDB),
                            R=[B_ytk, B_cb], W=[PSB[7]])
                    k.op("act", lambda e, tb=tb: e.activation(
                        out=yT[:, :, tb * 128:(tb + 1) * 128],
                        in_=pst[:, :].rearrange("p (c t) -> p c t", c=8), func=AF.Copy),
                        R=[PSB[7]], W=[B_yT])
                for oc in range(8):
                    bank = pp % 2
                    pp += 1
                    for fc in range(8):
                        k.op("pe", lambda e, oc=oc, fc=fc, bank=bank: e.matmul(
                            ps[bank][:, :], lhsT=Wo[:, fc, oc * 128:(oc + 1) * 128], rhs=yT[:, fc, :],
                            start=(fc == 0), stop=(fc == 7)), R=[B_Wo, B_yT], W=[PSB[bank]])
                    k.op("act", lambda e, oc=oc, bank=bank: e.activation(out=stage[:, oc, :], in_=ps[bank][:, :], func=AF.Copy),
                         R=[PSB[bank]], W=[B_stage])
                store_partial(T, stage, B_stage)
                reduce_scatter_tile(T)
            k.barrier()

    def phase_mlp(layer):
        last = layer == DEPTH - 1
        with contextlib.ExitStack() as es:
            def sb(name, shape, dt):
                return es.enter_context(nc.sbuf_tensor(un("ml_" + name), shape, dt))
            h = sb("h", [128, 8, TT], F32)
            dl = sb("d", [128, 8, TT], F32)
            hn = sb("hn", [128, 8, TT], BF16)
            sq = sb("sq", [128, 8, TT], BF16)
            tmp = sb("tmp", [128, TT], F32)
            rstd = sb("rstd", [128, TT], F32)
            u = sb("u", [128, 32, TT], BF16)
            rl = [sb(f"rl{i}", [128, TT], F32) for i in range(2)]
            wup = [sb(f"wup{i}", [128, 8, 1024], BF16) for i in range(2)]
            wdn = [sb(f"wdn{i}", [128, 8, 1024], BF16) for i in range(2)]
            ps = [es.enter_context(nc.psum_tensor(un(f"ml_ps{i}"), [128, TT], F32)) for i in range(8)]
            B_h, B_d, B_hn, B_sq, B_tmp, B_rstd, B_u = (Buf(n) for n in ("mh", "md", "mhn", "msq", "mtmp", "mrstd", "mu"))
            B_rl = [Buf("rl0"), Buf("rl1")]
            B_wup = [Buf("wup0"), Buf("wup1")]
            B_wdn = [Buf("wdn0"), Buf("wdn1")]
            wi = 0
            wj = 0
            pp = 0
            for j in range(NOWN):
                k.dma("sp", h[:, :, :], hres_d[j], R=[B_hres[j]], W=[B_h])
                for half in range(2):
                    m = 2 * j + half
                    k.dma("sp", dl[:, :, half * 256:(half + 1) * 256],
                          rb_d[m].ap().rearrange("(c p) t -> p c t", p=128), R=[B_rb[m]], W=[B_d], nowaw=True)
                k.op("pool", lambda e: e.tensor_tensor(out=h[:, :, :], in0=h[:, :, :], in1=dl[:, :, :], op=ALU.add),
                     R=[B_h, B_d], W=[B_h])
                rmsnorm(h, B_h, 4 + layer, hn, B_hn, sq, B_sq, tmp, B_tmp, rstd, B_rstd, ps[6], PSB[6])
                for q in range(4):
                    wb, B_wb = wup[wi % 2], B_wup[wi % 2]
                    wi += 1
                    load_w(wb, wup_d[layer][:, q * 1024:(q + 1) * 1024], 8, B_wb)
                    for hcl in range(8):
                        hc = q * 8 + hcl
                        bank = pp % 2
                        pp += 1
                        for kc in range(8):
                            k.op("pe", lambda e, kc=kc, hcl=hcl, bank=bank, wb=wb: e.matmul(
                                ps[bank][:, :], lhsT=wb[:, kc, hcl * 128:(hcl + 1) * 128], rhs=hn[:, kc, :],
                                start=(kc == 0), stop=(kc == 7)), R=[B_wb, B_hn], W=[PSB[bank]])
                        k.op("act", lambda e, bank=bank: e.activation(out=rl[bank][:, :], in_=ps[bank][:, :], func=AF.Relu),
                             R=[PSB[bank]], W=[B_rl[bank]])
                        k.op("dve", lambda e, bank=bank, hc=hc: e.tensor_tensor(
                            out=u[:, hc, :], in0=rl[bank][:, :], in1=rl[bank][:, :], op=ALU.mult),
                            R=[B_rl[bank]], W=[B_u])
                for q in range(4):
                    wb, B_wb = wdn[wj % 2], B_wdn[wj % 2]
                    wj += 1
                    load_w(wb, wdn_d[layer][q * 1024:(q + 1) * 1024, :], 8, B_wb)
                    for hcl in range(8):
                        hc = q * 8 + hcl
                        for oc in range(8):
                            k.op("pe", lambda e, hc=hc, hcl=hcl, oc=oc, wb=wb: e.matmul(
                                ps[oc][:, :], lhsT=wb[:, hcl, oc * 128:(oc + 1) * 128], rhs=u[:, hc, :],
                                start=(hc == 0), stop=(hc == 31)), R=[B_wb, B_u], W=[PSB[oc]])
                for oc in range(8):
                    k.op("dve", lambda e, oc=oc: e.tensor_tensor(
                        out=h[:, oc, :], in0=h[:, oc, :], in1=ps[oc][:, :], op=ALU.add),
                        R=[PSB[oc], B_h], W=[B_h])
                if not last:
                    k.dma("sp", hres_d[j], h[:, :, :], R=[B_h], W=[B_hres[j]])
                    rmsnorm(h, B_h, layer + 1, hn, B_hn, sq, B_sq, tmp, B_tmp, rstd, B_rstd, ps[6], PSB[6])
                    emit_hn(j, hn, B_hn)
                else:
                    rmsnorm(h, B_h, 8, dl, B_d, sq, B_sq, tmp, B_tmp, rstd, B_rstd, ps[6], PSB[6])
                    for tb in range(4):
                        for half in range(2):
                            bank = 4 + half
                            for cl in range(4):
                                c = half * 4 + cl
                                k.op("pe", lambda e, tb=tb, c=c, cl=cl, bank=bank: e.transpose(
                                    out=ps[bank][:, cl * 128:(cl + 1) * 128], in_=dl[:, c, tb * 128:(tb + 1) * 128],
                                    identity=IDF), R=[B_d, B_cf], W=[PSB[bank]])
                            k.op("act", lambda e, tb=tb, half=half, bank=bank: e.activation(
                                out=h[:, tb * 2 + half, :], in_=ps[bank][:, :], func=AF.Copy),
                                R=[PSB[bank]], W=[B_h])
                    k.dma("sp", y_d[j * TT:(j + 1) * TT, :].rearrange("(tb p) f -> p tb f", p=128),
                          h[:, :, :].rearrange("p (tb x) f -> p tb (x f)", x=2), R=[B_h], W=[B_y])
            k.barrier()

    B_y = Buf("yout")
    phases = [("p0", phase0)]
    for layer in range(DEPTH):
        if layer % 2 == 0:
            phases.append((f"mix{layer}", lambda layer=layer: phase_sb(layer // 2)))
        else:
            phases.append((f"mix{layer}", lambda layer=layer: phase_ret(layer // 2)))
        phases.append((f"mlp{layer}", lambda layer=layer: phase_mlp(layer)))
    for name, fn in phases:
        fn()
        if stop_after == name:
            break
    if debug:
        dbg_h = nc.dram_tensor("dbg_h", [NOWN, 128, 8, TT], F32, kind="ExternalOutput")
        dbg_yb = nc.dram_tensor("dbg_yb", [NOWN, 2 * D, TT], BF16, kind="ExternalOutput")
        dbg_rb = nc.dram_tensor("dbg_rb", [8, 8 * 128, 256], F32, kind="ExternalOutput")
        dbg_pb = nc.dram_tensor("dbg_pb", [8, 16 * 128, 256], F32, kind="ExternalOutput")
        B_dbg = Buf("dbg")
        k.barrier(engines=("sp",))
        for j in range(NOWN):
            k.dma("sp", dbg_h[j], hres_d[j], W=[B_dbg], nowaw=True)
            k.dma("sp", dbg_yb[j], yb_d[j].ap(), W=[B_dbg], nowaw=True)
        for m in range(8):
            k.dma("sp", dbg_rb[m], rb_d[m].ap(), W=[B_dbg], nowaw=True)
            k.dma("sp", dbg_pb[m], pb_d[m].ap(), W=[B_dbg], nowaw=True)
    k.barrier(engines=("sp",))
    return nc


def _consts(p):
    j = np.arange(128)
    cbm = np.zeros((128, NCB), np.float32)
    cbm[:, C_TRI:C_TRI + 128] = (j[:, None] >= j[None, :])
    cbm[:, C_ONES:C_ONES + 128] = 1.0
    cbm[:, C_MLE:C_MLE + 128] = (j[:, None] <= j[None, :])
    cbm[:, C_ID:C_ID + 128] = np.eye(128)
    t = np.arange(512)
    for d in range(4):
        cbm[:, C_MASK + d * 512:C_MASK + (d + 1) * 512] = (j[:, None] + 128 * d < t[None, :])
    cbm[:, C_UTRI:C_UTRI + 128] = (j[:, None] < j[None, :])
    return cbm


def _cf(p, g_mix, g_mlp, g_final):
    cfm = np.zeros((128, NCF), np.float32)
    cfm[:, F_ID:F_ID + 128] = np.eye(128)
    gains = np.concatenate([g_mix, g_mlp, g_final[None, :]], axis=0)
    cfm[:, F_G:F_G + 72] = gains.reshape(9, 8, 128).transpose(2, 0, 1).reshape(128, 72)
    j = np.arange(128, dtype=np.float64)
    for hl in range(2):
        hd = 2 * p + hl
        lg = np.log1p(-np.exp2(-5.0 - hd))
        cfm[:, F_SCW + hl] = np.exp(lg * (-1.0 - j)) / 16.0
        cfm[:, F_GC + hl] = np.exp(lg * 128.0)
        cfm[:, F_EPS + hl] = GN_EPS * np.exp(lg * (-2.0 * (j + 1.0)))
    return cfm


def _rope_tables():
    half = 128
    inv_freq = (1.0 / (10000.0 ** np.linspace(0.0, 1.0, half, dtype=np.float32))).astype(np.float32)
    pos = np.arange(S, dtype=np.float32)
    ang = (inv_freq[:, None] * pos[None, :]).astype(np.float32).astype(np.float64)
    return np.cos(ang).astype(np.float32), np.sin(ang).astype(np.float32)


_NC_CACHE = {}


def make_in_maps(x, w_sb_in, w_sb_out, w_ret_in, w_ret_out, g_mix, g_mlp, w_mlp_up, w_mlp_down, g_final):
    f = lambda a: np.ascontiguousarray(np.asarray(a, dtype=np.float32))
    x, w_sb_in, w_sb_out, w_ret_in, w_ret_out = f(x), f(w_sb_in), f(w_sb_out), f(w_ret_in), f(w_ret_out)
    g_mix, g_mlp, w_mlp_up, w_mlp_down, g_final = f(g_mix), f(g_mlp), f(w_mlp_up), f(w_mlp_down), f(g_final)
    cosT, sinT = _rope_tables()
    in_maps = []
    for c in range(8):
        b, p = c // 2, c % 2
        sl = slice(512 * p, 512 * p + 512)
        wsi = np.concatenate([w_sb_in[:, :, 0:1024][:, :, sl], w_sb_in[:, :, 1024:2048][:, :, sl],
                              w_sb_in[:, :, 2048:3072][:, :, sl]], axis=2)
        wso = w_sb_out[:, sl, :]
        sl2 = slice(1024 * p, 1024 * p + 1024)
        wri = np.concatenate([w_ret_in[:, :, 0:1024][:, :, sl], w_ret_in[:, :, 1024:2048][:, :, sl],
                              w_ret_in[:, :, 2048:4096][:, :, sl2], w_ret_in[:, :, 4096:6144][:, :, sl2]], axis=2)
        wro = w_ret_out[:, sl2, :]
        in_maps.append({
            "x": f(x[b, p * OWN:(p + 1) * OWN, :]),
            "w_sb_in": f(wsi), "w_sb_out": f(wso), "w_ret_in": f(wri), "w_ret_out": f(wro),
            "w_up": w_mlp_up, "w_dn": w_mlp_down,
            "cb": _consts(p), "cf": _cf(p, g_mix, g_mlp, g_final),
            "cosT": cosT, "sinT": sinT,
        })
    return in_maps


def kernel(x, w_sb_in, w_sb_out, w_ret_in, w_ret_out, g_mix, g_mlp, w_mlp_up, w_mlp_down, g_final):
    if "nc" not in _NC_CACHE:
        _NC_CACHE["nc"] = build_program()
    nc = _NC_CACHE["nc"]
    in_maps = make_in_maps(x, w_sb_in, w_sb_out, w_ret_in, w_ret_out, g_mix, g_mlp, w_mlp_up, w_mlp_down, g_final)
    res = run_bass_kernel_spmd(nc, in_maps, core_ids=list(range(8)))
    out = np.empty((NB, S, D), np.float32)
    for c in range(8):
        b, p = c // 2, c % 2
        out[b, p * OWN:(p + 1) * OWN, :] = np.asarray(res.results[c]["y"], dtype=np.float32)
    return out
```
